# Optimizing a Trainium2 kernel written in Bass

```python
import jax
import jax.numpy as jnp
from jax import lax
import numpy as np

D_MODEL = 1024
BATCH = 16
SEQ = 2048
DEPTH = 2

GRID_W = 64
CTX_LEN = 256
HEAD_DIM = 64
MLSTM_HEADS = D_MODEL // 256
NA_HEADS = D_MODEL // 128
CONV_CH = D_MODEL // 4
MLSTM_W = MLSTM_HEADS * HEAD_DIM
NA_W = NA_HEADS * HEAD_DIM
MIX_W = MLSTM_W + NA_W + CONV_CH
MLSTM_CHUNK = 64
NA_WIN_R = 8
NA_WIN_C = 16
CONV_WIDTH = 31
ROPE_BASE = 10000.0
N_EXPERTS = 32
TOP_K = 4
D_EXPERT = D_MODEL
SWIGLU_LIMIT = 7.0
SWIGLU_ALPHA = 1.702
MOE_BLOCK = 256
EPS = 1e-6

A_Q = 0
A_K = A_Q + MLSTM_W
A_V = A_K + MLSTM_W
A_O = A_V + MLSTM_W
A_G = A_O + MLSTM_W
B_Q = A_G + 4 * MLSTM_HEADS
B_K = B_Q + NA_W
B_V = B_K + NA_W
C_A = B_V + NA_W
C_G = C_A + CONV_CH
IN_COLS = C_G + CONV_CH

kernel_name = 'hymba_mlstm_natten_conformer_moe_dit'


def _rms(x, w):
    xf = x.astype(jnp.float32)
    y = xf * lax.rsqrt(jnp.mean(xf * xf, axis=-1, keepdims=True) + EPS)
    return (y * w.astype(jnp.float32)).astype(x.dtype)


def _layer_norm(x, w, b):
    xf = x.astype(jnp.float32)
    xc = xf - jnp.mean(xf, axis=-1, keepdims=True)
    y = xc * lax.rsqrt(jnp.mean(xc * xc, axis=-1, keepdims=True) + EPS)
    return (y * w.astype(jnp.float32) + b.astype(jnp.float32)).astype(x.dtype)


def _modulate(x, norm_w, shift, scale):
    return _rms(x, norm_w) * (1.0 + scale) + shift


def _rope_2d(x, rows, cols):
    half = HEAD_DIM // 2
    quarter = half // 2
    inv_freq = ROPE_BASE ** (-jnp.arange(quarter, dtype=jnp.float32) / quarter)

    def rot(xp, pos):
        ang = pos.astype(jnp.float32)[:, None] * inv_freq[None, :]
        cos = jnp.cos(ang)[:, None, :]
        sin = jnp.sin(ang)[:, None, :]
        x1, x2 = xp[..., :quarter], xp[..., quarter:]
        return jnp.concatenate([x1 * cos - x2 * sin, x1 * sin + x2 * cos], axis=-1)

    return jnp.concatenate([rot(x[..., :half], rows), rot(x[..., half:], cols)], axis=-1)


def _mlstm_chunkwise(q, k, v, ig, lf, state, with_output):
    Z, B, H, T, d = q.shape
    nc = T // MLSTM_CHUNK

    def chunks(a):
        a = a.reshape(a.shape[:3] + (nc, MLSTM_CHUNK) + a.shape[4:])
        return jnp.moveaxis(a, 3, 0)

    tril = jnp.tril(jnp.ones((MLSTM_CHUNK, MLSTM_CHUNK), dtype=bool))

    def step(carry, xs):
        C, n, m = carry
        qc, kc, vc, ic, fc = xs
        b = jnp.cumsum(fc, axis=-1)
        bL = b[..., -1]
        logw = bL[..., None] - b + ic
        m_new = jnp.maximum(bL + m, jnp.max(logw, axis=-1))
        w = jnp.exp(logw - m_new[..., None])
        decay = jnp.exp(bL + m - m_new)
        C_new = decay[..., None, None] * C + jnp.einsum('zbhs,zbhsk,zbhsv->zbhkv', w, kc, vc)
        n_new = decay[..., None] * n + jnp.einsum('zbhs,zbhsk->zbhk', w, kc)
        if not with_output:
            return (C_new, n_new, m_new), None
        logD = jnp.where(tril, b[..., :, None] - b[..., None, :] + ic[..., None, :], -jnp.inf)
        inter = b + m[..., None]
        m_t = jnp.maximum(inter, jnp.max(logD, axis=-1))
        s = jnp.einsum('zbhtd,zbhsd->zbhts', qc, kc) * jnp.exp(logD - m_t[..., None])
        w_inter = jnp.exp(inter - m_t)
        num = jnp.einsum('zbhts,zbhsd->zbhtd', s, vc) + w_inter[..., None] * jnp.einsum('zbhtk,zbhkv->zbhtv', qc, C)
        den = jnp.sum(s, axis=-1) + w_inter * jnp.einsum('zbhtk,zbhk->zbht', qc, n)
        h = num / jnp.maximum(jnp.abs(den), jnp.exp(-m_t))[..., None]
        return (C_new, n_new, m_new), h

    xs = (chunks(q), chunks(k), chunks(v), chunks(ig), chunks(lf))
    state, hs = lax.scan(step, state, xs)
    if not with_output:
        return None, state
    return jnp.moveaxis(hs, 0, 3).reshape(Z, B, H, T, d), state


def _mlstm_prep(P, ig_b, fg_b, pos):
    B, T = P.shape[:2]

    def heads(lo, hi):
        return P[..., lo:hi].astype(jnp.float32).reshape(B, T, MLSTM_HEADS, HEAD_DIM)

    q = heads(A_Q, A_K)
    k = heads(A_K, A_V) * (HEAD_DIM ** -0.5)
    v = heads(A_V, A_O)
    if pos is not None:
        q = _rope_2d(q, pos[0], pos[1])
        k = _rope_2d(k, pos[0], pos[1])
    g = P[..., A_G:B_Q].astype(jnp.float32).reshape(B, T, 4, MLSTM_HEADS)
    ig = g[:, :, 0:2] + ig_b.astype(jnp.float32)
    lf = jax.nn.log_sigmoid(g[:, :, 2:4] + fg_b.astype(jnp.float32))

    def dirs(a):
        a = jnp.swapaxes(a, 1, 2)
        return jnp.stack([a, jnp.flip(a, axis=2)])

    def gdirs(a):
        a = jnp.transpose(a, (2, 0, 3, 1))
        return jnp.stack([a[0], jnp.flip(a[1], axis=-1)])

    return dirs(q), dirs(k), dirs(v), gdirs(ig), gdirs(lf)


def _mlstm_merge(h, P, norm_w):
    B, T = P.shape[:2]
    hs = jnp.swapaxes(h[0] + jnp.flip(h[1], axis=2), 1, 2)
    hs = hs * lax.rsqrt(jnp.mean(hs * hs, axis=-1, keepdims=True) + EPS) * norm_w.astype(jnp.float32).reshape(MLSTM_HEADS, HEAD_DIM)
    o = jax.nn.sigmoid(P[..., A_O:A_G].astype(jnp.float32)).reshape(B, T, MLSTM_HEADS, HEAD_DIM)
    return (o * hs).reshape(B, T, MLSTM_W).astype(P.dtype)


def _mlstm_mixer(P_lat, P_ctx, pos, ig_b, fg_b, norm_w, need_ctx):
    qc, kc, vc, ic, fc = _mlstm_prep(P_ctx, ig_b, fg_b, None)
    Z, B, H, _, d = qc.shape
    state0 = (jnp.zeros((Z, B, H, d, d), jnp.float32), jnp.zeros((Z, B, H, d), jnp.float32), jnp.zeros((Z, B, H), jnp.float32))
    h_ctx, state_ctx = _mlstm_chunkwise(qc, kc, vc, ic, fc, state0, need_ctx)
    ql, kl, vl, il, fl = _mlstm_prep(P_lat, ig_b, fg_b, pos)
    h_lat, _ = _mlstm_chunkwise(ql, kl, vl, il, fl, state_ctx, True)
    out_ctx = _mlstm_merge(h_ctx, P_ctx, norm_w) if need_ctx else None
    return _mlstm_merge(h_lat, P_lat, norm_w), out_ctx


def _na_mixer(P_lat, P_ctx, qn_w, kn_w, rpb, need_ctx):
    B, S = P_lat.shape[:2]
    L = P_ctx.shape[1]
    scale = HEAD_DIM ** -0.5

    def heads(P, lo, hi):
        return P[..., lo:hi].reshape(P.shape[0], P.shape[1], NA_HEADS, HEAD_DIM)

    q = _rms(heads(P_lat, B_Q, B_K), qn_w) * scale
    k = _rms(heads(P_lat, B_K, B_V), kn_w)
    v = heads(P_lat, B_V, C_A)
    kc = _rms(heads(P_ctx, B_K, B_V), kn_w)
    vc = heads(P_ctx, B_V, C_A)
    out_ctx = None
    if need_ctx:
        qc = _rms(heads(P_ctx, B_Q, B_K), qn_w) * scale
        p = jax.nn.softmax(jnp.einsum('blhd,bmhd->bhlm', qc, kc).astype(jnp.float32), axis=-1).astype(vc.dtype)
        out_ctx = jnp.einsum('bhlm,bmhd->blhd', p, vc).reshape(B, L, NA_W)

    R = S // GRID_W
    KR = min(NA_WIN_R, R)

    def grid(a):
        return a.reshape(B, R, GRID_W, NA_HEADS, HEAD_DIM)

    kg, vg = grid(k), grid(v)
    q_rows = jnp.moveaxis(grid(q), 1, 0)
    col = jnp.arange(GRID_W)
    col_start = jnp.clip(col - NA_WIN_C // 2, 0, GRID_W - NA_WIN_C)
    in_win = (col[None, :] >= col_start[:, None]) & (col[None, :] < col_start[:, None] + NA_WIN_C)
    dc_idx = jnp.clip(col[None, :] - col[:, None] + NA_WIN_C - 1, 0, 2 * NA_WIN_C - 2)
    rpb_t = jnp.transpose(rpb[:, :, dc_idx], (0, 2, 1, 3)).astype(jnp.float32)

    def row_block(inp):
        r, q_r = inp
        r0 = jnp.clip(r - KR // 2, 0, R - KR)
        k_win = lax.dynamic_slice_in_dim(kg, r0, KR, axis=1)
        v_win = lax.dynamic_slice_in_dim(vg, r0, KR, axis=1)
        dr_idx = r0 + jnp.arange(KR) - r + NA_WIN_R - 1
        s_loc = jnp.einsum('bqhd,brkhd->bhqrk', q_r, k_win).astype(jnp.float32) + rpb_t[:, :, dr_idx][None]
        s_loc = jnp.where(in_win[:, None, :], s_loc, -jnp.inf)
        s_ctx = jnp.einsum('bqhd,blhd->bhql', q_r, kc).astype(jnp.float32)
        s_all = jnp.concatenate([s_loc.reshape(B, NA_HEADS, GRID_W, KR * GRID_W), s_ctx], axis=-1)
        p = jax.nn.softmax(s_all, axis=-1).astype(v.dtype)
        p_loc = p[..., :KR * GRID_W].reshape(B, NA_HEADS, GRID_W, KR, GRID_W)
        p_ctx = p[..., KR * GRID_W:]
        return jnp.einsum('bhqrk,brkhd->bqhd', p_loc, v_win) + jnp.einsum('bhql,blhd->bqhd', p_ctx, vc)

    out = lax.map(row_block, (jnp.arange(R, dtype=jnp.int32), q_rows))
    return jnp.moveaxis(out, 0, 1).reshape(B, S, NA_W), out_ctx


def _conv_module(P, conv_w, conv_b, ln_w, ln_b):
    u = P[..., C_A:C_G] * jax.nn.sigmoid(P[..., C_G:IN_COLS])
    y = lax.conv_general_dilated(u, conv_w[:, None, :], window_strides=(1,), padding='SAME',
                                 dimension_numbers=('NWC', 'WIO', 'NWC'), feature_group_count=CONV_CH) + conv_b
    y = _layer_norm(y, ln_w, ln_b)
    return y * jax.nn.sigmoid(y)


def _moe(xt, router_w, router_b, w1, b1, w2, b2):
    N, D = xt.shape
    NK = N * TOP_K
    logits = (xt @ router_w + router_b).astype(jnp.float32)
    top_val, top_idx = lax.top_k(logits, TOP_K)
    gates = jax.nn.softmax(top_val, axis=-1)
    flat_e = top_idx.reshape(-1)
    flat_tok = jnp.repeat(jnp.arange(N, dtype=jnp.int32), TOP_K)
    flat_g = gates.reshape(-1)
    order = jnp.argsort(flat_e)
    se, stok, sg = flat_e[order], flat_tok[order], flat_g[order]
    counts = jnp.bincount(flat_e, length=N_EXPERTS)
    padded = (counts + MOE_BLOCK - 1) // MOE_BLOCK * MOE_BLOCK
    pad_end = jnp.cumsum(padded)
    pad_start = pad_end - padded
    start = jnp.cumsum(counts) - counts
    dest = pad_start[se] + jnp.arange(NK, dtype=jnp.int32) - start[se]
    n_blocks = -(-(NK + N_EXPERTS * (MOE_BLOCK - 1)) // MOE_BLOCK)
    P = n_blocks * MOE_BLOCK
    row_tok = jnp.full((P,), N, jnp.int32).at[dest].set(stok)
    row_g = jnp.zeros((P,), jnp.float32).at[dest].set(sg)
    block_e = jnp.minimum(jnp.searchsorted(pad_end, jnp.arange(n_blocks, dtype=jnp.int32) * MOE_BLOCK, side='right'), N_EXPERTS - 1)
    x_pad = jnp.concatenate([xt, jnp.zeros((1, D), xt.dtype)], axis=0)

    def block(y, inp):
        tok, g, e = inp
        h = x_pad[tok] @ w1[e] + b1[e]
        glu = jnp.minimum(h[:, :D_EXPERT], SWIGLU_LIMIT)
        lin = jnp.clip(h[:, D_EXPERT:], -SWIGLU_LIMIT, SWIGLU_LIMIT)
        act = (lin + 1.0) * glu * jax.nn.sigmoid(SWIGLU_ALPHA * glu)
        out = act @ w2[e] + b2[e]
        return y.at[tok].add(g[:, None] * out.astype(jnp.float32)), None

    y, _ = lax.scan(block, jnp.zeros((N + 1, D), jnp.float32),
                    (row_tok.reshape(n_blocks, MOE_BLOCK), row_g.reshape(n_blocks, MOE_BLOCK), block_e))
    return y[:N].astype(xt.dtype)


def _layer(x, ctx, c, c_ctx, norm_mix_w, norm_ffn_w, w_ada, b_ada, w_in, mlstm_ig_b, mlstm_fg_b, mlstm_norm_w,
           na_qnorm_w, na_knorm_w, na_rpb, conv_w, conv_b, conv_ln_w, conv_ln_b, w_out, router_w, router_b,
           exp_w1, exp_b1, exp_w2, exp_b2, pos, need_ctx):
    B, S, D = x.shape
    L = ctx.shape[1]
    sh1, sc1, g1, sh2, sc2, g2 = [m[:, None, :] for m in jnp.split(jax.nn.silu(c) @ w_ada + b_ada, 6, axis=-1)]
    csh1, csc1, cg1, csh2, csc2, cg2 = jnp.split(jax.nn.silu(c_ctx) @ w_ada + b_ada, 6, axis=-1)
    P = _modulate(x, norm_mix_w, sh1, sc1) @ w_in
    Pc = _modulate(ctx, norm_mix_w, csh1, csc1) @ w_in
    a_x, a_c = _mlstm_mixer(P, Pc, pos, mlstm_ig_b, mlstm_fg_b, mlstm_norm_w, need_ctx)
    b_x, b_c = _na_mixer(P, Pc, na_qnorm_w, na_knorm_w, na_rpb, need_ctx)
    c_x = _conv_module(P, conv_w, conv_b, conv_ln_w, conv_ln_b)
    x = x + g1 * (jnp.concatenate([a_x, b_x, c_x], axis=-1) @ w_out)
    hx = _modulate(x, norm_ffn_w, sh2, sc2)
    if need_ctx:
        c_c = _conv_module(Pc, conv_w, conv_b, conv_ln_w, conv_ln_b)
        ctx = ctx + cg1 * (jnp.concatenate([a_c, b_c, c_c], axis=-1) @ w_out)
        hc = _modulate(ctx, norm_ffn_w, csh2, csc2)
        y = _moe(jnp.concatenate([hx.reshape(B * S, D), hc.reshape(B * L, D)], axis=0),
                 router_w, router_b, exp_w1, exp_b1, exp_w2, exp_b2)
        x = x + g2 * y[:B * S].reshape(B, S, D)
        ctx = ctx + cg2 * y[B * S:].reshape(B, L, D)
    else:
        x = x + g2 * _moe(hx.reshape(B * S, D), router_w, router_b, exp_w1, exp_b1, exp_w2, exp_b2).reshape(B, S, D)
    return x, ctx


def setup_inputs(seed: int = 0) -> dict:
    key = jax.random.key(seed)
    ks = jax.random.split(key, 26)
    nrm = jax.random.normal
    f32 = jnp.float32
    D = D_MODEL
    return {
        'x': nrm(ks[0], (BATCH, SEQ, D), f32),
        'c': nrm(ks[1], (BATCH, D), f32),
        'ctx': nrm(ks[2], (BATCH, CTX_LEN, D), f32),
        'c_ctx': nrm(ks[3], (D,), f32),
        'norm_mix_w': 1.0 + 0.02 * nrm(ks[4], (DEPTH, D), f32),
        'norm_ffn_w': 1.0 + 0.02 * nrm(ks[5], (DEPTH, D), f32),
        'w_ada': nrm(ks[6], (DEPTH, D, 6 * D), f32) * (0.5 * D ** -0.5),
        'b_ada': 0.02 * nrm(ks[7], (DEPTH, 6 * D), f32),
        'w_in': nrm(ks[8], (DEPTH, D, IN_COLS), f32) * D ** -0.5,
        'mlstm_ig_b': 0.1 * nrm(ks[9], (DEPTH, 2, MLSTM_HEADS), f32),
        'mlstm_fg_b': jnp.linspace(3.0, 6.0, MLSTM_HEADS, dtype=f32)[None, None, :] + 0.1 * nrm(ks[10], (DEPTH, 2, MLSTM_HEADS), f32),
        'mlstm_norm_w': 1.0 + 0.02 * nrm(ks[11], (DEPTH, MLSTM_W), f32),
        'na_qnorm_w': 1.0 + 0.02 * nrm(ks[12], (DEPTH, HEAD_DIM), f32),
        'na_knorm_w': 1.0 + 0.02 * nrm(ks[13], (DEPTH, HEAD_DIM), f32),
        'na_rpb': 0.02 * nrm(ks[14], (DEPTH, NA_HEADS, 2 * NA_WIN_R - 1, 2 * NA_WIN_C - 1), f32),
        'conv_w': nrm(ks[15], (DEPTH, CONV_WIDTH, CONV_CH), f32) * CONV_WIDTH ** -0.5,
        'conv_b': 0.02 * nrm(ks[16], (DEPTH, CONV_CH), f32),
        'conv_ln_w': 1.0 + 0.02 * nrm(ks[17], (DEPTH, CONV_CH), f32),
        'conv_ln_b': 0.02 * nrm(ks[18], (DEPTH, CONV_CH), f32),
        'w_out': nrm(ks[19], (DEPTH, MIX_W, D), f32) * MIX_W ** -0.5,
        'router_w': nrm(ks[20], (DEPTH, D, N_EXPERTS), f32) * D ** -0.5,
        'router_b': 0.01 * nrm(ks[21], (DEPTH, N_EXPERTS), f32),
        'exp_w1': nrm(ks[22], (DEPTH, N_EXPERTS, D, 2 * D_EXPERT), f32) * D ** -0.5,
        'exp_b1': 0.02 * nrm(ks[23], (DEPTH, N_EXPERTS, 2 * D_EXPERT), f32),
        'exp_w2': nrm(ks[24], (DEPTH, N_EXPERTS, D_EXPERT, D), f32) * D_EXPERT ** -0.5,
        'exp_b2': 0.02 * nrm(ks[25], (DEPTH, N_EXPERTS, D), f32),
    }


def reference(x, c, ctx, c_ctx, norm_mix_w, norm_ffn_w, w_ada, b_ada, w_in, mlstm_ig_b, mlstm_fg_b, mlstm_norm_w,
              na_qnorm_w, na_knorm_w, na_rpb, conv_w, conv_b, conv_ln_w, conv_ln_b, w_out, router_w, router_b,
              exp_w1, exp_b1, exp_w2, exp_b2):
    S = x.shape[1]
    t = jnp.arange(S, dtype=jnp.int32)
    pos = (t // GRID_W, t % GRID_W)
    for l in range(DEPTH):
        x, ctx = _layer(x, ctx, c, c_ctx, norm_mix_w[l], norm_ffn_w[l], w_ada[l], b_ada[l], w_in[l],
                        mlstm_ig_b[l], mlstm_fg_b[l], mlstm_norm_w[l], na_qnorm_w[l], na_knorm_w[l], na_rpb[l],
                        conv_w[l], conv_b[l], conv_ln_w[l], conv_ln_b[l], w_out[l], router_w[l], router_b[l],
                        exp_w1[l], exp_b1[l], exp_w2[l], exp_b2[l], pos, l < DEPTH - 1)
    return x
```

```python
from contextlib import ExitStack

import ml_dtypes
import numpy as np

import concourse.bass as bass
import concourse.mybir as mybir
from concourse.bass_utils import run_bass_kernel_spmd

F32 = mybir.dt.float32
BF16 = mybir.dt.bfloat16
AF = mybir.ActivationFunctionType
ALU = mybir.AluOpType
AX = mybir.AxisListType

D = 1024
SEQ = 2048
CTX = 256
T = SEQ + CTX
NT = T // 128
DEPTH = 2
NE = 32
IN_COLS = 3088
TOKC = 2576
A_Q, A_K, A_V, A_O, A_G = 0, 256, 512, 768, 1024
B_Q, B_K, B_V = 1040, 1552, 2064
EPS = 1e-6
NEG = -30000.0
GROUPS = [(0, 256, 1), (256, 512, 0), (768, 512, 0), (1280, 512, 0), (1792, 512, 0)]


class Sched:
    CE = ("pe", "dve", "act", "pool")
    NSLOT = 8

    def __init__(self, nc, es):
        self.nc = nc
        self.es = es
        self.eng = {"pe": nc.tensor, "dve": nc.vector, "act": nc.scalar, "pool": nc.gpsimd, "sp": nc.sync}
        self.ops = []
        self.last_w = {}
        self.readers = {}
        self.epoch = 0

    def _rec(self, kind, eng, fn, r, w, kw):
        idx = len(self.ops)
        deps = set()
        for k in r:
            p = self.last_w.get(k)
            if p is not None:
                deps.add(p)
        for k in w:
            p = self.last_w.get(k)
            if p is not None:
                deps.add(p)
            deps.update(self.readers.get(k, ()))
        for k in r:
            lst = self.readers.setdefault(k, [])
            if kind == "c":
                lst[:] = [j for j in lst if not (self.ops[j]["kind"] == "c" and self.ops[j]["eng"] == eng)]
            lst.append(idx)
        for k in w:
            self.last_w[k] = idx
            self.readers[k] = []
        deps.discard(idx)
        op = dict(kind=kind, eng=eng, fn=fn, kw=kw, deps=deps, sig=False, epoch=self.epoch)
        for d in deps:
            po = self.ops[d]
            if po["epoch"] == self.epoch and not (po["eng"] == "pe" and eng == "pe" and po["kind"] == "c" and kind == "c"):
                po["sig"] = True
        self.ops.append(op)
        return idx

    def op(self, eng, fn, r=(), w=(), **kw):
        return self._rec("c", eng, fn, r, w, kw)

    def dma(self, q, out, in_, r=(), w=()):
        return self._rec("d", q, "dma_start", r, w, dict(out=out, in_=in_))

    def barrier(self):
        self.ops.append(dict(kind="b", epoch=self.epoch))
        self.epoch += 1

    def emit(self):
        nc = self.nc
        sems = None
        cnt = dcnt = waited = dwaited = None
        slots = {q: [self.es.enter_context(nc.semaphore(f"dq_{q}{i}")) for i in range(self.NSLOT)] for q in ("sp", "pool")}
        dissued = {"sp": 0, "pool": 0}
        allq = self.CE + ("sp",)

        sems = {e: self.es.enter_context(nc.semaphore(f"s_{e}")) for e in self.CE}
        cnt = {e: 0 for e in self.CE}
        waited = {q: {e: 0 for e in self.CE} for q in allq}
        dwaited = {q: {} for q in allq}

        def new_epoch(ep):
            pass

        new_epoch(0)
        last = {}
        for i, o in enumerate(self.ops):
            if o["kind"] == "b":
                for e, j in last.items():
                    self.ops[j]["sig"] = True
                last = {}
            elif o["kind"] == "c":
                last[o["eng"]] = i
        for e, j in last.items():
            self.ops[j]["sig"] = True

        TR = False

        def wait_dma(q, key, val):
            if dwaited[q].get(key, 0) < val:
                self.eng[q].wait_ge(slots[key[0]][key[1]], val)
                dwaited[q][key] = val
                if TR:
                    print(f"[{q}] wait dma{key} >= {val}")

        def wait_eng(q, e, val):
            if waited[q][e] < val:
                self.eng[q].wait_ge(sems[e], val)
                waited[q][e] = val
                if TR:
                    print(f"[{q}] wait {e} >= {val}")

        ep = 0
        for o in self.ops:
            if o["kind"] == "b":
                for q in allq:
                    for e in self.CE:
                        wait_eng(q, e, cnt[e])
                    for dq in ("sp", "pool"):
                        n = dissued[dq]
                        for s in range(self.NSLOT):
                            nuse = (n - s + self.NSLOT - 1) // self.NSLOT if n > s else 0
                            if nuse:
                                wait_dma(q, (dq, s), 16 * nuse)
                ep += 1
                new_epoch(ep)
                continue
            q = o["eng"]
            for d in sorted(o["deps"]):
                p = self.ops[d]
                if p["epoch"] != ep:
                    continue
                if p["kind"] == "d":
                    wait_dma(q, p["dkey"], p["dval"])
                elif p["sig"]:
                    wait_eng(q, p["eng"], p["sigval"])
            if o["kind"] == "d":
                n = dissued[q]
                s = n % self.NSLOT
                use = n // self.NSLOT
                if use > 0:
                    wait_dma(q, (q, s), 16 * use)
                ins = self.eng[q].dma_start(**o["kw"])
                ins.then_inc(slots[q][s], 16)
                o["dkey"] = (q, s)
                o["dval"] = 16 * (use + 1)
                dissued[q] = n + 1
                if TR:
                    print(f"[{q}] DMA -> dma{(q, s)} = {o['dval']}  out={o['kw']['out'].tensor.name}")
            else:
                ins = getattr(self.eng[q], o["fn"])(**o["kw"])
                if o["sig"]:
                    ins.then_inc(sems[q], 1)
                    cnt[q] += 1
                    o["sigval"] = cnt[q]
                if TR:
                    print(f"[{q}] {o['fn']} sig={o.get('sigval')} out={[v.tensor.name for k, v in o['kw'].items() if k in ('out', 'ap')]}")
        for q in allq:
            for e in self.CE:
                wait_eng(q, e, cnt[e])
            for dq in ("sp", "pool"):
                n = dissued[dq]
                for s in range(self.NSLOT):
                    nuse = (n - s + self.NSLOT - 1) // self.NSLOT if n > s else 0
                    if nuse:
                        wait_dma(q, (dq, s), 16 * nuse)


def _consts():
    c = {}
    c["ident_f"] = np.eye(128, dtype=np.float32)
    c["ident_b"] = np.eye(128, dtype=np.float32).astype(ml_dtypes.bfloat16)
    c["ones_b"] = np.ones((128, 128), np.float32).astype(ml_dtypes.bfloat16)
    c["ones_f"] = np.ones((128, 128), np.float32)
    u = np.arange(128)
    triu = (u[:, None] <= u[None, :]).astype(np.float32)
    tril = (u[:, None] >= u[None, :]).astype(np.float32)
    c["tri"] = np.stack([triu] * 4 + [tril] * 4, axis=1).astype(np.float32)
    nm = np.zeros((128, 2, 4, 128), np.float32)
    nm[:, 0, :, :] = np.where(u[:, None] > u[None, :], NEG, 0.0)[:, None, :]
    nm[:, 1, :, :] = np.where(u[:, None] < u[None, :], NEG, 0.0)[:, None, :]
    c["negmask"] = nm.reshape(128, 2, 512)
    quarter = 16
    inv = (10000.0 ** (-np.arange(quarter, dtype=np.float32) / quarter)).astype(np.float32)
    t = np.arange(SEQ)
    rows = (t // 64).astype(np.float32)
    cols = (t % 64).astype(np.float32)
    ang = np.stack([rows[:, None] * inv[None, :], cols[:, None] * inv[None, :]], axis=1).astype(np.float32)
    cs, sn = np.cos(ang).astype(np.float32), np.sin(ang).astype(np.float32)
    C = np.ones((T, 2, 4, 2, 16), np.float32)
    Sn = np.zeros((T, 2, 4, 2, 16), np.float32)
    C[CTX:] = cs[:, None, None, :, :]
    Sn[CTX:] = sn[:, None, None, :, :]
    C[:, 1] *= 0.125
    Sn[:, 1] *= 0.125
    c["ropeC"] = C.reshape(T, 256)
    c["ropeS"] = Sn.reshape(T, 256)
    col = np.arange(64)
    cstart = np.clip(col - 8, 0, 48)
    inw = (col[None, :] >= cstart[:, None]) & (col[None, :] < cstart[:, None] + 16)
    cm = inw.T.astype(np.float32)
    c["cmask"] = np.concatenate([cm, cm], axis=0)
    return c


CONST_SPECS = [("ident_f", [128, 128], F32), ("ident_b", [128, 128], BF16), ("ones_b", [128, 128], BF16),
               ("ones_f", [128, 128], F32), ("tri", [128, 8, 128], F32), ("negmask", [128, 2, 512], F32),
               ("ropeC", [T, 256], F32), ("ropeS", [T, 256], F32), ("cmask", [128, 64], F32)]

IN_SPECS = [("x", [2, SEQ, D]), ("ctx", [2, CTX, D]), ("cs", [2, 2, D]),
            ("norm_mix_w", [DEPTH, D]), ("norm_ffn_w", [DEPTH, D]), ("w_ada", [DEPTH, D, 6 * D]), ("b_ada", [DEPTH, 6 * D]),
            ("w_in", [DEPTH, D, IN_COLS]), ("mlstm_ig_b", [DEPTH, 8]), ("mlstm_fg_b", [DEPTH, 8]),
            ("mlstm_norm_w", [DEPTH, 256]), ("na_qnorm_w", [DEPTH, 64]), ("na_knorm_w", [DEPTH, 64]),
            ("rpbT", [DEPTH, 8, 15 * 64, 64]), ("conv_w", [DEPTH, 31, 256]), ("conv_b", [DEPTH, 256]),
            ("conv_ln_w", [DEPTH, 256]), ("conv_ln_b", [DEPTH, 256]), ("w_out", [DEPTH, D, D]),
            ("router_w", [DEPTH, D, NE]), ("router_b", [DEPTH, NE]), ("exp_w1", [DEPTH, NE, D, 2 * D]),
            ("exp_b1", [DEPTH, NE, 2 * D]), ("exp_w2", [DEPTH, NE, D, D]), ("exp_b2", [DEPTH, NE, D])]


class Ctx:
    pass


def build_program(stop_after=None, n_batch=2, depth=DEPTH, dbg=None, ne=NE):
    nc = bass.Bass("TRN2", target_bir_lowering=False)
    es = ExitStack()
    S = Sched(nc, es)
    g = Ctx()
    g.nc, g.S, g.ne = nc, S, ne
    g.inp = {n: nc.dram_tensor(n, s, F32, kind="ExternalInput") for n, s in IN_SPECS}
    g.cin = {n: nc.dram_tensor(n, s, dt, kind="ExternalInput") for n, s, dt in CONST_SPECS}
    g.y = nc.dram_tensor("y", [2, SEQ, D], F32, kind="ExternalOutput")
    g.dbg = {}
    for n, s in (dbg or []):
        g.dbg[n] = nc.dram_tensor(n, s, F32, kind="ExternalOutput")
    g.xT_d = [nc.dram_tensor(f"xT_d{b}", [D, T], F32) for b in range(2)]
    g.ptok_d = [nc.dram_tensor(f"ptok_d{b}", [T, TOKC], F32) for b in range(2)]
    g.pcT_d = [nc.dram_tensor(f"pcT_d{b}", [512, T], F32) for b in range(2)]
    g.mix_d = [nc.dram_tensor(f"mix_d{b}", [T, D], F32) for b in range(2)]
    g.gate_d = [nc.dram_tensor(f"gate_d{b}", [32, T], F32) for b in range(2)]

    uid = [0]

    def sb(name, shape, dt=F32, scope=None):
        uid[0] += 1
        return (scope or es).enter_context(nc.sbuf_tensor(f"{name}_u{uid[0]}", shape, dt))

    g.sb = sb
    g.c = {}
    for n, s, dt in CONST_SPECS:
        if n in ("ropeC", "ropeS", "tri", "negmask", "cmask"):
            continue
        g.c[n] = sb("c_" + n, s, dt)
        S.dma("sp", g.c[n][:], g.cin[n][:], w=[("c", n)])
    g.ps = [es.enter_context(nc.psum_tensor(f"ps{i}", [128, 512], F32)) for i in range(8)]
    g.modT = sb("modT", [128, 48, 2])
    g.a1 = sb("a1", [128, 8, 2])
    g.a2 = sb("a2", [128, 8, 2])
    S.barrier()

    done = False
    for b in range(n_batch):
        phase_start(g, b)
        S.barrier()
        for l in range(depth):
            need_ctx = l < DEPTH - 1
            for ph in (phase_ada, phase_proj, phase_na, phase_mlstm, phase_conv, phase_ffn):
                if ph is phase_ada:
                    pre = ExitStack()
                    g.win = sb("pj_win", [128, 8, IN_COLS], BF16, pre)
                    wsrc = g.inp["w_in"][l].rearrange("(c p) f -> p c f", p=128)
                    for i, (c0, c1) in enumerate(((0, 772), (772, 1544), (1544, 2316), (2316, IN_COLS))):
                        S.dma("pool", g.win[:, :, c0:c1], wsrc[:, :, c0:c1], w=[("pj_win", i)])
                ph(g, l, b, need_ctx)
                S.barrier()
                if ph is phase_proj:
                    pre.close()
                if stop_after == (ph.__name__, l, b):
                    done = True
                    break
            if done:
                break
        if done:
            break
    if dbg:
        with ExitStack() as sc:
            buf = sb("dbgbuf", [128, 4096], F32, sc)
            for n, s in dbg:
                src = {"ptok": g.ptok_d[0], "pcT": g.pcT_d[0], "mix": g.mix_d[0], "xT": g.xT_d[0]}[n]
                rows, cols = s
                for r0 in range(0, rows, 128):
                    S.dma("sp", buf[:, 0:cols], src[r0:r0 + 128, :], r=[("d", src.name)], w=["dbgbuf"])
                    S.dma("sp", g.dbg[n][r0:r0 + 128, :], buf[:, 0:cols], r=["dbgbuf"], w=[("d", n)])
    with nc.allow_non_contiguous_dma(reason="small per-feature vector loads / layout changes"):
        S.emit()
    es.close()
    return nc


def rsqrt_inplace(g, ap, keys):
    g.S.op("act", "activation", out=ap, in_=ap, func=AF.Sqrt, r=keys, w=keys)
    g.S.op("dve", "reciprocal", out=ap, in_=ap, r=keys, w=keys)


def evac(g, i, out, in_, r, w):
    if i % 2 == 0:
        g.S.op("act", "activation", out=out, in_=in_, func=AF.Copy, r=r, w=w)
    else:
        g.S.op("dve", "tensor_copy", out=out, in_=in_, r=r, w=w)


def phase_start(g, b):
    S, nc = g.S, g.nc
    with ExitStack() as sc:
        xt = [g.sb(f"st_x{i}", [128, D], F32, sc) for i in range(2)]
        xo = [g.sb(f"st_o{i}", [128, 8, 128], F32, sc) for i in range(2)]
        dst = g.xT_d[b].rearrange("(c p) t -> p c t", p=128)
        for ti in range(NT):
            s = ti % 2
            src = g.inp["ctx"][b, ti * 128:(ti + 1) * 128, :] if ti < 2 else g.inp["x"][b, (ti - 2) * 128:(ti - 1) * 128, :]
            S.dma("sp", xt[s][:], src, w=[("st_x", s)])
            for h in range(2):
                pb = g.ps[(ti * 2 + h) % 8]
                for j in range(4):
                    cc = h * 4 + j
                    S.op("pe", "transpose", out=pb[:, j * 128:(j + 1) * 128], in_=xt[s][:, cc * 128:(cc + 1) * 128],
                         identity=g.c["ident_f"][:], r=[("st_x", s), ("c", "ident_f")], w=[("ps", (ti * 2 + h) % 8)])
                evac(g, h, xo[s][:, h * 4:(h + 1) * 4, :], pb[:, :].rearrange("p (j t) -> p j t", j=4),
                     r=[("ps", (ti * 2 + h) % 8)], w=[("st_o", s, h)])
            S.dma("sp", dst[:, :, ti * 128:(ti + 1) * 128], xo[s][:], r=[("st_o", s, 0), ("st_o", s, 1)], w=[("d", "xT", b)])


def load_vec(g, dst, src_1d, n, w, q="sp"):
    g.S.dma(q, dst, src_1d.rearrange("(c p o) -> p c o", p=128, o=1), w=w)


def phase_ada(g, l, b, need_ctx):
    S = g.S
    with ExitStack() as sc:
        scT = g.sb("ada_sc", [128, 8, 2], F32, sc)
        brow = g.sb("ada_brow", [2, 6 * D], F32, sc)
        mrow = g.sb("ada_mrow", [2, 6 * D], F32, sc)
        nw = g.sb("ada_nw", [128, 8, 2], F32, sc)
        tmp = g.sb("ada_tmp", [128, 8, 2], F32, sc)
        wa = [g.sb(f"ada_w{i}", [128, 8, 512], F32, sc) for i in range(2)]
        for j in range(2):
            load_vec(g, scT[:, :, j:j + 1], g.inp["cs"][b, j], 8, w=["ada_sc"])
        S.dma("sp", brow[:], g.inp["b_ada"][l].partition_broadcast(2), w=["ada_brow"])
        load_vec(g, nw[:, :, 0:1], g.inp["norm_mix_w"][l], 8, w=["ada_nw"])
        load_vec(g, nw[:, :, 1:2], g.inp["norm_ffn_w"][l], 8, w=["ada_nw"])
        S.op("act", "activation", out=scT[:], in_=scT[:], func=AF.Silu, r=["ada_sc"], w=["ada_sc"])
        wsrc = g.inp["w_ada"][l].rearrange("(c p) f -> p c f", p=128)
        for pc in range(12):
            s = pc % 2
            bk = 1 + pc % 4
            S.dma("sp", wa[s][:], wsrc[:, :, pc * 512:(pc + 1) * 512], w=[("ada_w", s)])
            for kc in range(8):
                S.op("pe", "matmul", out=g.ps[bk][0:2, :], lhsT=scT[:, kc, :], rhs=wa[s][:, kc, :], start=(kc == 0), stop=(kc == 7),
                     r=[("ada_w", s), "ada_sc"], w=[("ps", bk)])
            S.op("dve", "tensor_tensor", out=mrow[:, pc * 512:(pc + 1) * 512], in0=g.ps[bk][0:2, :], in1=brow[:, pc * 512:(pc + 1) * 512], op=ALU.add,
                 r=[("ps", bk), "ada_brow"], w=[("ada_mrow", pc)])
        pm = g.ps[0]
        for fc in range(48):
            S.op("pe", "transpose", out=pm[:, fc * 2:fc * 2 + 2], in_=mrow[0:2, fc * 128:(fc + 1) * 128], identity=g.c["ident_f"][0:2, 0:2],
                 r=[("ada_mrow", fc // 4), ("c", "ident_f")], w=[("ps", 0)])
        S.op("dve", "tensor_copy", out=g.modT[:], in_=pm[:, 0:96].rearrange("p (f j) -> p f j", j=2), r=[("ps", 0)], w=["modT"])
        for which, dst, k in ((1, g.a1, 0), (4, g.a2, 1)):
            S.op("dve", "tensor_scalar", out=tmp[:], in0=g.modT[:, which * 8:(which + 1) * 8, :], scalar1=1.0, scalar2=None,
                 op0=ALU.add, r=["modT"], w=["ada_tmp"])
            S.op("dve", "tensor_tensor", out=dst[:], in0=tmp[:], in1=nw[:, :, k:k + 1].to_broadcast([128, 8, 2]), op=ALU.mult,
                 r=["ada_tmp", "ada_nw"], w=[("a", k)])


def phase_proj(g, l, b, need_ctx):
    S = g.S
    with ExitStack() as sc:
        win = g.win
        xg = [g.sb(f"pj_xg{i}", [128, 8, 512], F32, sc) for i in range(2)]
        sq = g.sb("pj_sq", [128, 8, 512], BF16, sc)
        rstd = g.sb("pj_rstd", [128, 512], F32, sc)
        tmp = [g.sb(f"pj_tmp{i}", [128, 512], F32, sc) for i in range(2)]
        xm = [g.sb(f"pj_xm{i}", [128, 8, 512], BF16, sc) for i in range(2)]
        ptok = [g.sb(f"pj_ptok{i}", [128, TOKC], F32, sc) for i in range(2)]
        pcT = g.sb("pj_pcT", [128, 4, 512], F32, sc)
        winr = [("pj_win", i) for i in range(4)]
        xsrc = g.xT_d[b].rearrange("(c p) t -> p c t", p=128)
        pcdst = g.pcT_d[b].rearrange("(c p) t -> p c t", p=128)
        colg = [(0, 512), (512, 512), (1024, 512), (1536, 512), (2048, 512), (2560, 16)]
        st = dict(nbank=0, tcount=0)

        def prologue(gi):
            t0, W, j = GROUPS[gi]
            s = gi % 2
            S.dma("sp", xg[s][:, :, 0:W], xsrc[:, :, t0:t0 + W], r=[("d", "xT", b)], w=[("pj_xg", s)])
            S.op("act", "activation", out=sq[:, :, 0:W], in_=xg[s][:, :, 0:W], func=AF.Square, r=[("pj_xg", s)], w=["pj_sq"])
            for kc in range(8):
                S.op("pe", "matmul", out=g.ps[0][:, 0:W], lhsT=g.c["ones_b"][:], rhs=sq[:, kc, 0:W], start=(kc == 0), stop=(kc == 7),
                     r=["pj_sq", ("c", "ones_b")], w=[("ps", 0)])
            S.op("dve", "tensor_scalar", out=rstd[:, 0:W], in0=g.ps[0][:, 0:W], scalar1=1.0 / D, scalar2=EPS, op0=ALU.mult, op1=ALU.add,
                 r=[("ps", 0)], w=["pj_rstd"])
            rsqrt_inplace(g, rstd[:, 0:W], ["pj_rstd"])
            for kc in range(8):
                ts = kc % 2
                S.op("dve", "tensor_tensor", out=tmp[ts][:, 0:W], in0=xg[s][:, kc, 0:W], in1=rstd[:, 0:W], op=ALU.mult,
                     r=[("pj_xg", s), "pj_rstd"], w=[("pj_tmp", ts)])
                S.op("act", "activation", out=xm[s][:, kc, 0:W], in_=tmp[ts][:, 0:W], func=AF.Identity, scale=g.a1[:, kc, j:j + 1],
                     bias=g.modT[:, 0 * 8 + kc, j:j + 1], r=[("pj_tmp", ts), ("a", 0), "modT"], w=[("pj_xm", s, kc)])

        def project(gi):
            t0, W, j = GROUPS[gi]
            s = gi % 2
            xmr = [("pj_xm", s, kc) for kc in range(8)]
            for tt in range(W // 128):
                ps_ = st["tcount"] % 2
                st["tcount"] += 1
                for ci, (c0, cw) in enumerate(colg):
                    bk = 1 + st["nbank"] % 7
                    st["nbank"] += 1
                    for kc in range(8):
                        S.op("pe", "matmul", out=g.ps[bk][:, 0:cw], lhsT=xm[s][:, kc, tt * 128:(tt + 1) * 128], rhs=win[:, kc, c0:c0 + cw],
                             start=(kc == 0), stop=(kc == 7), r=xmr + winr, w=[("ps", bk)])
                    evac(g, ci, ptok[ps_][:, c0:c0 + cw], g.ps[bk][:, 0:cw], r=[("ps", bk)], w=[("pj_ptok", ps_, ci)])
                S.dma("sp", g.ptok_d[b][t0 + tt * 128:t0 + (tt + 1) * 128, :], ptok[ps_][:],
                      r=[("pj_ptok", ps_, ci) for ci in range(6)], w=[("d", "ptok", b)])
            for fc in range(4):
                bk = 1 + st["nbank"] % 7
                st["nbank"] += 1
                for kc in range(8):
                    S.op("pe", "matmul", out=g.ps[bk][:, 0:W], lhsT=win[:, kc, TOKC + fc * 128:TOKC + (fc + 1) * 128], rhs=xm[s][:, kc, 0:W],
                         start=(kc == 0), stop=(kc == 7), r=xmr + winr, w=[("ps", bk)])
                evac(g, fc, pcT[:, fc, 0:W], g.ps[bk][:, 0:W], r=[("ps", bk)], w=[("pj_pcT", fc)])
            S.dma("sp", pcdst[:, :, t0:t0 + W], pcT[:, :, 0:W], r=[("pj_pcT", fc) for fc in range(4)], w=[("d", "pcT", b)])

        prologue(0)
        for gi in range(len(GROUPS)):
            if gi + 1 < len(GROUPS):
                prologue(gi + 1)
            project(gi)


def phase_conv(g, l, b, need_ctx):
    S = g.S
    with ExitStack() as sc:
        cw = g.sb("cv_w", [128, 2, 31], F32, sc)
        cb = g.sb("cv_b", [128, 2, 1], F32, sc)
        lnw = g.sb("cv_lnw", [128, 256], F32, sc)
        lnb = g.sb("cv_lnb", [128, 256], F32, sc)
        for c in range(2):
            S.dma("sp", cw[:, c, :], g.inp["conv_w"][l, :, c * 128:(c + 1) * 128].rearrange("j p -> p j"), w=["cv_w"])
        load_vec(g, cb[:], g.inp["conv_b"][l], 2, w=["cv_b"])
        S.dma("sp", lnw[:], g.inp["conv_ln_w"][l].partition_broadcast(128), w=["cv_lnw"])
        S.dma("sp", lnb[:], g.inp["conv_ln_b"][l].partition_broadcast(128), w=["cv_lnb"])
        at = [g.sb(f"cv_a{c}", [128, SEQ], F32, sc) for c in range(2)]
        gt = [g.sb(f"cv_g{c}", [128, SEQ], F32, sc) for c in range(2)]
        up = [g.sb(f"cv_u{c}", [128, SEQ + 30], F32, sc) for c in range(2)]
        yy = [g.sb(f"cv_y{c}", [128, SEQ], F32, sc) for c in range(2)]
        yt = [g.sb(f"cv_yt{i}", [128, 256], F32, sc) for i in range(2)]
        xc = [g.sb(f"cv_xc{i}", [128, 256], F32, sc) for i in range(2)]
        st = [g.sb(f"cv_st{i}", [128, 4], F32, sc) for i in range(2)]
        segs = ([(0, CTX)] if need_ctx else []) + [(CTX, SEQ)]
        nt = 0
        for (t0, n) in segs:
            for c in range(2):
                ve = "dve"
                S.dma("sp", at[c][:, 0:n], g.pcT_d[b][c * 128:(c + 1) * 128, t0:t0 + n], r=[("d", "pcT", b)], w=[("cv_a", c)])
                S.dma("sp", gt[c][:, 0:n], g.pcT_d[b][256 + c * 128:256 + (c + 1) * 128, t0:t0 + n], r=[("d", "pcT", b)], w=[("cv_g", c)])
                S.op("act", "activation", out=gt[c][:, 0:n], in_=gt[c][:, 0:n], func=AF.Sigmoid, r=[("cv_g", c)], w=[("cv_g", c)])
                S.op(ve, "memset", ap=up[c][:, 0:15], constant=0.0, w=[("cv_u", c)])
                S.op(ve, "memset", ap=up[c][:, 15 + n:30 + n], constant=0.0, r=[("cv_u", c)], w=[("cv_u", c)])
                S.op(ve, "tensor_tensor", out=up[c][:, 15:15 + n], in0=at[c][:, 0:n], in1=gt[c][:, 0:n], op=ALU.mult,
                     r=[("cv_a", c), ("cv_g", c), ("cv_u", c)], w=[("cv_u", c)])
                S.op(ve, "tensor_scalar", out=yy[c][:, 0:n], in0=up[c][:, 0:n], scalar1=cw[:, c, 0:1], scalar2=cb[:, c, :], op0=ALU.mult,
                     op1=ALU.add, r=[("cv_u", c), "cv_w", "cv_b"], w=[("cv_y", c)])
                for j in range(1, 31):
                    if ve == "dve":
                        S.op(ve, "scalar_tensor_tensor", out=yy[c][:, 0:n], in0=up[c][:, j:j + n], scalar=cw[:, c, j:j + 1], in1=yy[c][:, 0:n],
                             op0=ALU.mult, op1=ALU.add, r=[("cv_u", c), "cv_w", ("cv_y", c)], w=[("cv_y", c)])
                    else:
                        S.op(ve, "tensor_scalar", out=at[c][:, 0:n], in0=up[c][:, j:j + n], scalar1=cw[:, c, j:j + 1], scalar2=None, op0=ALU.mult,
                             r=[("cv_u", c), "cv_w", ("cv_a", c)], w=[("cv_a", c)])
                        S.op(ve, "tensor_tensor", out=yy[c][:, 0:n], in0=yy[c][:, 0:n], in1=at[c][:, 0:n], op=ALU.add,
                             r=[("cv_a", c), ("cv_y", c)], w=[("cv_y", c)])
            for tt in range(n // 128):
                i = nt % 2
                bk = nt % 8
                nt += 1
                for c in range(2):
                    S.op("pe", "transpose", out=g.ps[bk][:, c * 128:(c + 1) * 128], in_=yy[c][:, tt * 128:(tt + 1) * 128], identity=g.c["ident_f"][:],
                         r=[("cv_y", c), ("c", "ident_f")], w=[("ps", bk)])
                S.op("act", "activation", out=yt[i][:], in_=g.ps[bk][:, 0:256], func=AF.Copy, r=[("ps", bk)], w=[("cv_yt", i)])
                layer_norm_swish(g, yt[i], xc[i], st[i], lnw, lnb, ("cv_yt", i), ("cv_xc", i), ("cv_st", i))
                S.dma("sp", g.mix_d[b][t0 + tt * 128:t0 + (tt + 1) * 128, 768:1024], yt[i][:], r=[("cv_yt", i)], w=[("d", "mix", b)])


def layer_norm_swish(g, yt, xc, st, lnw, lnb, ky, kx, ks):
    S = g.S
    S.op("dve", "tensor_reduce", out=st[:, 0:1], in_=yt[:], axis=AX.X, op=ALU.add, r=[ky], w=[ks])
    S.op("dve", "tensor_scalar", out=st[:, 0:1], in0=st[:, 0:1], scalar1=-1.0 / 256, scalar2=None, op0=ALU.mult, r=[ks], w=[ks])
    S.op("dve", "tensor_scalar", out=xc[:], in0=yt[:], scalar1=st[:, 0:1], scalar2=None, op0=ALU.add, r=[ky, ks], w=[kx])
    S.op("dve", "tensor_tensor", out=yt[:], in0=xc[:], in1=xc[:], op=ALU.mult, r=[kx], w=[ky])
    S.op("dve", "tensor_reduce", out=st[:, 1:2], in_=yt[:], axis=AX.X, op=ALU.add, r=[ky], w=[ks])
    S.op("dve", "tensor_scalar", out=st[:, 1:2], in0=st[:, 1:2], scalar1=1.0 / 256, scalar2=EPS, op0=ALU.mult, op1=ALU.add, r=[ks], w=[ks])
    rsqrt_inplace(g, st[:, 1:2], [ks])
    S.op("dve", "tensor_scalar", out=xc[:], in0=xc[:], scalar1=st[:, 1:2], scalar2=None, op0=ALU.mult, r=[kx, ks], w=[kx])
    S.op("dve", "tensor_tensor", out=xc[:], in0=xc[:], in1=lnw[:], op=ALU.mult, r=[kx, "cv_lnw"], w=[kx])
    S.op("dve", "tensor_tensor", out=xc[:], in0=xc[:], in1=lnb[:], op=ALU.add, r=[kx, "cv_lnb"], w=[kx])
    S.op("act", "activation", out=yt[:], in_=xc[:], func=AF.Sigmoid, r=[kx], w=[ky])
    S.op("dve", "tensor_tensor", out=yt[:], in0=yt[:], in1=xc[:], op=ALU.mult, r=[kx, ky], w=[ky])


def phase_na(g, l, b, need_ctx):
    S, nc = g.S, g.nc
    with ExitStack() as sc:
        qw = g.sb("na_qw", [128, 64], F32, sc)
        kw_ = g.sb("na_kw", [128, 64], F32, sc)
        wbc = g.sb("na_wbc", [128, 16, 64], F32, sc)
        cm = g.sb("na_cmask", [128, 64], F32, sc)
        S.dma("sp", cm[:], g.cin["cmask"][:], w=[("c", "cmask")])
        etmp = g.sb("na_etmp", [128, 14, 64], F32, sc)
        etab = g.sb("na_etab", [128, 8, 14, 64], BF16, sc)
        qT = g.sb("na_qT", [128, 8, T], BF16, sc)
        kT = g.sb("na_kT", [128, 4, T], BF16, sc)
        vE = g.sb("na_vE", [128, NT, 8, 65], BF16, sc)
        vO = g.sb("na_vO", [128, 15, 8, 65], BF16, sc)
        pt = [g.sb(f"na_pt{i}", [128, 1536], F32, sc) for i in range(2)]
        sq = g.sb("na_sq", [128, 1024], F32, sc)
        ms = g.sb("na_ms", [128, 16], F32, sc)
        qkn = g.sb("na_qkn", [128, 1024], BF16, sc)
        pv = [g.sb(f"na_pv{i}", [128, 512], F32, sc) for i in range(2)]
        ex = [g.sb(f"na_ex{i}", [128, 384], BF16, sc) for i in range(3)]
        pm = [g.sb(f"na_pm{i}", [128, 256], BF16, sc) for i in range(3)]
        rd = [g.sb(f"na_rd{i}", [64, 8], F32, sc) for i in range(2)]
        ona = [g.sb(f"na_o{i}", [64, 8, 64], F32, sc) for i in range(2)]
        S.dma("sp", qw[:], g.inp["na_qnorm_w"][l].partition_broadcast(128), w=["na_qw"])
        S.dma("sp", kw_[:], g.inp["na_knorm_w"][l].partition_broadcast(128), w=["na_kw"])
        S.op("dve", "tensor_scalar", out=wbc[:, 0:8, :], in0=qw[:].unsqueeze(1).to_broadcast([128, 8, 64]), scalar1=0.125, scalar2=None,
             op0=ALU.mult, r=["na_qw"], w=["na_wbc"])
        S.op("dve", "tensor_copy", out=wbc[:, 8:16, :], in_=kw_[:].unsqueeze(1).to_broadcast([128, 8, 64]), r=["na_kw", "na_wbc"], w=["na_wbc"])
        S.op("pool", "memset", ap=vE[:, :, :, 64:65], constant=1.0, w=["na_vE1"])
        S.op("pool", "memset", ap=qT[:], constant=0.0, w=["na_qT"])
        S.op("pool", "memset", ap=vO[:, :, :, 64:65], constant=1.0, w=["na_vO1"])
        rp = g.inp["rpbT"]
        for h in range(8):
            src = bass.AP(rp, (l * 8 + h) * 960 * 64, [[64, 128], [64 * 64, 14], [1, 64]])
            S.dma("sp", etmp[:], src, w=["na_etmp"])
            S.op("act", "activation", out=etmp[:], in_=etmp[:], func=AF.Exp, r=["na_etmp"], w=["na_etmp"])
            S.op("dve", "tensor_tensor", out=etab[:, h, :, :], in0=etmp[:], in1=cm[:].unsqueeze(1).to_broadcast([128, 14, 64]), op=ALU.mult,
                 r=["na_etmp", ("c", "cmask")], w=[("na_etab", h)])
        for ti in range(NT):
            s = ti % 2
            S.dma("sp", pt[s][:], g.ptok_d[b][ti * 128:(ti + 1) * 128, B_Q:TOKC], r=[("d", "ptok", b)], w=[("na_pt", s)])
            S.op("act", "activation", out=sq[:], in_=pt[s][:, 0:1024], func=AF.Square, r=[("na_pt", s)], w=["na_sq"])
            S.op("dve", "tensor_reduce", out=ms[:], in_=sq[:].rearrange("p (h d) -> p h d", d=64), axis=AX.X, op=ALU.add, r=["na_sq"], w=["na_ms"])
            S.op("dve", "tensor_scalar", out=ms[:], in0=ms[:], scalar1=1.0 / 64, scalar2=EPS, op0=ALU.mult, op1=ALU.add, r=["na_ms"], w=["na_ms"])
            rsqrt_inplace(g, ms[:], ["na_ms"])
            S.op("dve", "tensor_tensor", out=sq[:].rearrange("p (h d) -> p h d", d=64), in0=pt[s][:, 0:1024].rearrange("p (h d) -> p h d", d=64),
                 in1=ms[:].unsqueeze(2).to_broadcast([128, 16, 64]), op=ALU.mult, r=[("na_pt", s), "na_ms", "na_sq"], w=["na_sq"])
            S.op("pool", "tensor_tensor", out=qkn[:].rearrange("p (h d) -> p h d", d=64), in0=sq[:].rearrange("p (h d) -> p h d", d=64),
                 in1=wbc[:], op=ALU.mult, r=["na_sq", "na_wbc"], w=["na_qkn"])
            bk = ti % 2
            pbf = g.ps[bk].bitcast(BF16)
            for j in range(8):
                S.op("pe", "transpose", out=pbf[:, j * 128:(j + 1) * 128], in_=qkn[:, j * 128:(j + 1) * 128], identity=g.c["ident_b"][:],
                     r=["na_qkn", ("c", "ident_b")], w=[("ps", bk)])
            S.op("act", "activation", out=qT[0:64, 0:8:2, ti * 128:(ti + 1) * 128], in_=pbf[0:64, 0:512].rearrange("p (j t) -> p j t", j=4), func=AF.Copy,
                 r=[("ps", bk), "na_qT"], w=["na_qT"])
            S.op("dve", "tensor_copy", out=qT[64:128, 1:8:2, ti * 128:(ti + 1) * 128], in_=pbf[64:128, 0:512].rearrange("p (j t) -> p j t", j=4),
                 r=[("ps", bk), "na_qT"], w=["na_qT"])
            S.op("dve", "tensor_copy", out=kT[:, :, ti * 128:(ti + 1) * 128], in_=pbf[:, 512:1024].rearrange("p (j t) -> p j t", j=4),
                 r=[("ps", bk)], w=["na_kT"])
            S.op("pool", "tensor_copy", out=vE[:, ti, :, 0:64], in_=pt[s][:, 1024:1536].rearrange("p (h d) -> p h d", d=64),
                 r=[("na_pt", s), "na_vE1"], w=["na_vE"])
        for j in range(15):
            s = j % 2
            r0 = CTX + 64 + j * 128
            S.dma("sp", pv[s][:], g.ptok_d[b][r0:r0 + 128, B_V:TOKC], r=[("d", "ptok", b)], w=[("na_pv", s)])
            S.op("pool", "tensor_copy", out=vO[:, j, :, 0:64], in_=pv[s][:].rearrange("p (h d) -> p h d", d=64), r=[("na_pv", s), "na_vO1"], w=["na_vO"])
        units = []
        if need_ctx:
            for qb in range(4):
                units.append((qb * 64, qb * 64, []))
        for r in range(32):
            r0 = min(max(r - 4, 0), 24)
            loc = []
            for j in range(4):
                gr = r0 + 2 * j
                vt = vE[:, 2 + gr // 2] if gr % 2 == 0 else vO[:, (gr - 1) // 2]
                loc.append((CTX + gr * 64, vt))
            units.append((CTX + r * 64, CTX + r * 64, loc, r0 - r + 7))
        nu = 0
        na_r = ["na_qT", "na_kT", "na_vE", "na_vO", "na_vE1", "na_vO1"]
        items = [(ui, h) for ui in range(len(units)) for h in range(8)]

        def tiles_of(ui):
            loc = units[ui][2]
            return [(k0, vt) for (k0, vt) in loc] + [(0, vE[:, 0]), (128, vE[:, 1])], len(loc)

        def scores(i):
            ui, h = items[i]
            tq = units[ui][0]
            pr = h // 2
            sb_, xi = i % 4, i % 3
            tiles, nl = tiles_of(ui)
            nk = len(tiles)
            for jj, (k0, vt) in enumerate(tiles):
                S.op("pe", "matmul", out=g.ps[sb_][:, jj * 64:(jj + 1) * 64], lhsT=kT[:, pr, k0:k0 + 128], rhs=qT[:, h, tq:tq + 64],
                     start=True, stop=True, r=na_r, w=[("ps", sb_)])
            S.op("act", "activation", out=ex[xi][:, 0:nk * 64], in_=g.ps[sb_][:, 0:nk * 64], func=AF.Exp, r=[("ps", sb_)], w=[("na_ex", xi)])
            if nl:
                d0 = units[ui][3]
                S.op("dve", "tensor_tensor", out=pm[xi][:].rearrange("p (j q) -> p j q", j=4), in0=ex[xi][:, 0:256].rearrange("p (j q) -> p j q", j=4),
                     in1=etab[:, h, d0:d0 + 7:2, :], op=ALU.mult, r=[("na_ex", xi), ("na_etab", h)], w=[("na_pm", xi)])

        def pv(i):
            ui, h = items[i]
            xi = i % 3
            ob = (4 + 2 * (ui % 2), 5 + 2 * (ui % 2))
            tiles, nl = tiles_of(ui)
            nk = len(tiles)
            pso = g.ps[ob[h // 4]]
            oc = (h % 4) * 65
            for jj, (k0, vt) in enumerate(tiles):
                lhs = pm[xi][:, jj * 64:(jj + 1) * 64] if jj < nl else ex[xi][:, jj * 64:(jj + 1) * 64]
                S.op("pe", "matmul", out=pso[0:64, oc:oc + 65], lhsT=lhs, rhs=vt[:, h, :], start=(jj == 0), stop=(jj == nk - 1),
                     r=[("na_ex", xi), ("na_pm", xi)] + na_r, w=[("ps", ob[h // 4])])
            if h == 7:
                trow = units[ui][1]
                oi = ui % 2
                for hh in range(2):
                    psq = g.ps[ob[hh]]
                    v3 = psq[0:64, 0:260].rearrange("p (h d) -> p h d", d=65)
                    S.op("dve", "reciprocal", out=rd[oi][:, hh * 4:(hh + 1) * 4], in_=v3[:, :, 64], r=[("ps", ob[hh])], w=[("na_rd", oi, hh)])
                    S.op("dve", "tensor_tensor", out=ona[oi][:, hh * 4:(hh + 1) * 4, :], in0=v3[:, :, 0:64],
                         in1=rd[oi][:, hh * 4:(hh + 1) * 4].unsqueeze(2).to_broadcast([64, 4, 64]), op=ALU.mult,
                         r=[("ps", ob[hh]), ("na_rd", oi, hh)], w=[("na_o", oi, hh)])
                S.dma("sp", g.mix_d[b][trow:trow + 64, 256:768], ona[oi][:].rearrange("p h d -> p (h d)"), r=[("na_o", oi, 0), ("na_o", oi, 1)],
                      w=[("d", "mix", b)])

        scores(0)
        for i in range(len(items)):
            if i + 1 < len(items):
                scores(i + 1)
            pv(i)


def phase_mlstm(g, l, b, need_ctx):
    S = g.S
    with ExitStack() as sc:
        qk = g.sb("ml_qk", [128, NT, 512], BF16, sc)
        g.c["tri"] = g.sb("ml_tri", [128, 8, 128], F32, sc)
        g.c["negmask"] = g.sb("ml_negmask", [128, 2, 512], F32, sc)
        S.dma("sp", g.c["tri"][:], g.cin["tri"][:], w=[("c", "tri")])
        S.dma("sp", g.c["negmask"][:], g.cin["negmask"][:], w=[("c", "negmask")])
        vx = g.sb("ml_vx", [128, NT, 4, 65], BF16, sc)
        osig = g.sb("ml_osig", [128, NT, 256], F32, sc)
        gl = g.sb("ml_gl", [128, NT, 16], F32, sc)
        cum = g.sb("ml_cum", [128, NT, 16], F32, sc)
        hh = [g.sb(f"ml_h{d}", [128, NT, 256], F32, sc) for d in range(2)]
        qkT = g.sb("ml_qkT", [128, NT, 4, 128], BF16, sc)
        qm = g.sb("ml_qm", [128, NT, 4, 128], BF16, sc)
        igb = g.sb("ml_igb", [128, 8], F32, sc)
        fgb = g.sb("ml_fgb", [128, 8], F32, sc)
        nwb = g.sb("ml_nwb", [128, 256], F32, sc)
        pt = [g.sb(f"ml_pt{i}", [128, 1040], F32, sc) for i in range(2)]
        rc = [g.sb(f"ml_rc{i}", [128, 256], F32, sc) for i in range(2)]
        rs = [g.sb(f"ml_rs{i}", [128, 256], F32, sc) for i in range(2)]
        t0_ = g.sb("ml_t0", [128, 16, 16], F32, sc)
        t1_ = g.sb("ml_t1", [128, 16, 16], F32, sc)
        t2_ = g.sb("ml_t2", [128, 16, 16], F32, sc)
        t3_ = g.sb("ml_t3", [128, 16, 16], F32, sc)
        zz = g.sb("ml_zz", [128, 8], F32, sc)
        S.dma("sp", igb[:], g.inp["mlstm_ig_b"][l].partition_broadcast(128), w=["ml_igb"])
        S.dma("sp", fgb[:], g.inp["mlstm_fg_b"][l].partition_broadcast(128), w=["ml_fgb"])
        S.dma("sp", nwb[:], g.inp["mlstm_norm_w"][l].partition_broadcast(128), w=["ml_nwb"])
        S.op("pool", "memset", ap=vx[:, :, :, 64:65], constant=1.0, w=["ml_vx1"])
        S.op("pool", "memset", ap=qm[:], constant=0.0, w=["ml_qm0"])
        for ti in range(NT):
            s = ti % 2
            S.dma("sp", pt[s][:], g.ptok_d[b][ti * 128:(ti + 1) * 128, 0:1040], r=[("d", "ptok", b)], w=[("ml_pt", s)])
            S.dma("sp", rc[s][:], g.cin["ropeC"][ti * 128:(ti + 1) * 128, :], w=[("ml_rc", s)])
            S.dma("sp", rs[s][:], g.cin["ropeS"][ti * 128:(ti + 1) * 128, :], w=[("ml_rs", s)])
            x4 = pt[s][:, 0:512].rearrange("p (g x i) -> p g x i", x=2, i=16)
            x0, x1 = x4[:, :, 0, :], x4[:, :, 1, :]
            C = rc[s][:].rearrange("p (g i) -> p g i", i=16)
            Sn = rs[s][:].rearrange("p (g i) -> p g i", i=16)
            o4 = qk[:, ti, :].rearrange("p (g x i) -> p g x i", x=2, i=16)
            kr = [("ml_pt", s), ("ml_rc", s), ("ml_rs", s)]
            S.op("dve", "tensor_tensor", out=t0_[:], in0=x0, in1=C, op=ALU.mult, r=kr, w=["ml_t0"])
            S.op("pool", "tensor_tensor", out=t1_[:], in0=x1, in1=Sn, op=ALU.mult, r=kr, w=["ml_t1"])
            S.op("dve", "tensor_tensor", out=o4[:, :, 0, :], in0=t0_[:], in1=t1_[:], op=ALU.subtract, r=["ml_t0", "ml_t1"], w=[("ml_qk", ti, 0)])
            S.op("pool", "tensor_tensor", out=t2_[:], in0=x1, in1=C, op=ALU.mult, r=kr, w=["ml_t2"])
            S.op("dve", "tensor_tensor", out=t3_[:], in0=x0, in1=Sn, op=ALU.mult, r=kr, w=["ml_t3"])
            S.op("pool", "tensor_tensor", out=o4[:, :, 1, :], in0=t2_[:], in1=t3_[:], op=ALU.add, r=["ml_t2", "ml_t3"], w=[("ml_qk", ti, 1)])
            S.op("pool", "tensor_copy", out=vx[:, ti, :, 0:64], in_=pt[s][:, 512:768].rearrange("p (h d) -> p h d", d=64),
                 r=[("ml_pt", s), "ml_vx1"], w=[("ml_vx", ti)])
            S.op("act", "activation", out=osig[:, ti, :], in_=pt[s][:, 768:1024], func=AF.Sigmoid, r=[("ml_pt", s)], w=[("ml_osig", ti)])
            S.op("dve", "tensor_tensor", out=gl[:, ti, 0:8], in0=pt[s][:, 1024:1032], in1=igb[:], op=ALU.add, r=[("ml_pt", s), "ml_igb"], w=[("ml_gl", ti)])
            S.op("dve", "tensor_tensor", out=zz[:], in0=pt[s][:, 1032:1040], in1=fgb[:], op=ALU.add, r=[("ml_pt", s), "ml_fgb"], w=["ml_zz"])
            S.op("act", "activation", out=zz[:], in_=zz[:], func=AF.Exp, scale=-1.0, r=["ml_zz"], w=["ml_zz"])
            S.op("act", "activation", out=zz[:], in_=zz[:], func=AF.Ln, bias=1.0, r=["ml_zz"], w=["ml_zz"])
            S.op("dve", "tensor_scalar", out=gl[:, ti, 8:16], in0=zz[:], scalar1=-1.0, scalar2=None, op0=ALU.mult, r=["ml_zz", ("ml_gl", ti)], w=[("ml_gl", ti)])
            bk = ti % 2
            pb = g.ps[bk]
            S.op("pe", "matmul", out=pb[:, 0:4], lhsT=g.c["tri"][:, 0, :], rhs=gl[:, ti, 8:12], start=True, stop=True, r=[("ml_gl", ti), ("c", "tri")], w=[("ps", bk)])
            S.op("pe", "matmul", out=pb[:, 4:8], lhsT=g.c["tri"][:, 4, :], rhs=gl[:, ti, 12:16], start=True, stop=True, r=[("ml_gl", ti), ("c", "tri")], w=[("ps", bk)])
            S.op("pe", "matmul", out=pb[:, 8:16], lhsT=g.c["ones_f"][:], rhs=gl[:, ti, 8:16], start=True, stop=True, r=[("ml_gl", ti), ("c", "ones_f")], w=[("ps", bk)])
            S.op("dve", "tensor_copy", out=cum[:, ti, :], in_=pb[:, 0:16], r=[("ps", bk)], w=[("ml_cum", ti)])
            bk2 = 2 + ti % 2
            pbf = g.ps[bk2].bitcast(BF16)
            for j in range(4):
                S.op("pe", "transpose", out=pbf[:, j * 128:(j + 1) * 128], in_=qk[:, ti, j * 128:(j + 1) * 128], identity=g.c["ident_b"][:],
                     r=[("ml_qk", ti, 0), ("ml_qk", ti, 1), ("c", "ident_b")], w=[("ps", bk2)])
            S.op("act", "activation", out=qkT[:, ti, :, :], in_=pbf[:, 0:512].rearrange("p (j t) -> p j t", j=4), func=AF.Copy, r=[("ps", bk2)], w=[("ml_qkT", ti)])
            S.op("act", "activation", out=qm[0:64, ti, 0:4:2, :], in_=pbf[0:64, 0:256].rearrange("p (j t) -> p j t", j=2), func=AF.Copy, r=[("ps", bk2), "ml_qm0"], w=[("ml_qm", ti, 0)])
            S.op("act", "activation", out=qm[64:128, ti, 1:4:2, :], in_=pbf[64:128, 0:256].rearrange("p (j t) -> p j t", j=2), func=AF.Copy, r=[("ps", bk2), "ml_qm0"], w=[("ml_qm", ti, 1)])
        sc4 = [[g.sb(f"ml_sc{d}{i}", [128, 16], F32, sc) for i in range(2)] for d in range(2)]
        rmat = [[g.sb(f"ml_rmat{d}{i}", [128, 4, 128], F32, sc) for i in range(2)] for d in range(2)]
        AT = [[g.sb(f"ml_AT{d}{i}", [128, 4, 128], F32, sc) for i in range(2)] for d in range(2)]
        Sm = [[g.sb(f"ml_Sm{d}{i}", [128, 4, 128], BF16, sc) for i in range(2)] for d in range(2)]
        qs = [[g.sb(f"ml_qs{d}{i}", [128, 256], BF16, sc) for i in range(2)] for d in range(2)]
        ks = [[g.sb(f"ml_ks{d}{i}", [128, 256], BF16, sc) for i in range(2)] for d in range(2)]
        qsT = [[g.sb(f"ml_qsT{d}{i}", [128, 2, 128], BF16, sc) for i in range(2)] for d in range(2)]
        Cf = [g.sb(f"ml_Cf{d}", [128, 2, 130], F32, sc) for d in range(2)]
        Cb = [[g.sb(f"ml_Cb{d}{i}", [128, 2, 130], BF16, sc) for i in range(2)] for d in range(2)]
        rd = [g.sb(f"ml_rd{d}", [128, 4], F32, sc) for d in range(2)]
        orders = [list(range(NT)), [1, 0] + list(range(NT - 1, 1, -1))]
        for d in range(2):
            S.op("pool", "memset", ap=Cf[d][:], constant=0.0, w=[("ml_Cf", d)])
            S.op("pool", "memset", ap=Cb[d][0][:], constant=0.0, w=[("ml_Cb", d, 0)])

        def pre(idx, d):
            ti = orders[d][idx]
            first = idx == 0
            i2 = idx % 2
            sc_ = sc4[d][i2]
            ksc = ("ml_sc", d, i2)
            bcol = cum[:, ti, d * 4:(d + 1) * 4]
            bL = cum[:, ti, 8 + d * 4:12 + d * 4]
            ig = gl[:, ti, d * 4:(d + 1) * 4]
            rr = [("ml_cum", ti), ("ml_gl", ti)]
            S.op("dve", "tensor_tensor", out=sc_[:, 4:8], in0=bL, in1=bcol, op=ALU.subtract, r=rr, w=[ksc])
            S.op("dve", "tensor_tensor", out=sc_[:, 4:8], in0=sc_[:, 4:8], in1=ig, op=ALU.add, r=rr + [ksc], w=[ksc])
            S.op("dve", "tensor_tensor", out=sc_[:, 8:12], in0=ig, in1=bcol, op=ALU.subtract, r=rr + [ksc], w=[ksc])
            S.op("dve", "tensor_copy", out=sc_[:, 0:4], in_=bcol, r=rr + [ksc], w=[ksc])
            S.op("dve", "tensor_copy", out=sc_[:, 12:16], in_=bL, r=rr + [ksc], w=[ksc])
            S.op("act", "activation", out=sc_[:, 0:8], in_=sc_[:, 0:8], func=AF.Exp, r=[ksc], w=[ksc])
            S.op("act", "activation", out=sc_[:, 12:16], in_=sc_[:, 12:16], func=AF.Exp, r=[ksc], w=[ksc])
            S.op("dve", "tensor_tensor", out=rmat[d][i2][:], in0=g.c["tri"][:, d * 4:(d + 1) * 4, :],
                 in1=gl[:, ti, 8 + d * 4:12 + d * 4].unsqueeze(2).to_broadcast([128, 4, 128]), op=ALU.mult, r=[("ml_gl", ti), ("c", "tri")], w=[("ml_rmat", d, i2)])
            ba = d
            S.op("pe", "matmul", out=g.ps[ba][:, :], lhsT=g.c["ones_f"][:], rhs=rmat[d][i2][:].rearrange("p h t -> p (h t)"), start=True, stop=False,
                 r=[("ml_rmat", d, i2), ("c", "ones_f")], w=[("ps", ba)])
            S.op("pe", "matmul", out=g.ps[ba][:, :], lhsT=g.c["ident_f"][:],
                 rhs=g.c["negmask"][:, d, :], start=False, stop=True, r=[("c", "negmask"), ("c", "ident_f")], w=[("ps", ba)])
            for h in range(4):
                S.op("act", "activation", out=AT[d][i2][:, h, :], in_=g.ps[ba][:, h * 128:(h + 1) * 128], func=AF.Exp, bias=sc_[:, 8 + h:9 + h],
                     r=[("ps", ba), ksc], w=[("ml_AT", d, i2)])
            bs = 2 + d
            for h in range(4):
                pr = h // 2
                S.op("pe", "matmul", out=g.ps[bs][:, h * 128:(h + 1) * 128], lhsT=qkT[:, ti, 2 + pr, :], rhs=qm[:, ti, h, :],
                     start=True, stop=True, r=[("ml_qkT", ti), ("ml_qm", ti, 0), ("ml_qm", ti, 1)], w=[("ps", bs)])
            S.op("dve", "tensor_tensor", out=Sm[d][i2][:], in0=g.ps[bs][:, :].rearrange("p (h t) -> p h t", h=4), in1=AT[d][i2][:], op=ALU.mult,
                 r=[("ps", bs), ("ml_AT", d, i2)], w=[("ml_Sm", d, i2)])
            qkr = [("ml_qk", ti, 0), ("ml_qk", ti, 1)]
            S.op("pool", "tensor_tensor", out=ks[d][i2][:].rearrange("p (h x) -> p h x", h=4), in0=qk[:, ti, 256:512].rearrange("p (h x) -> p h x", h=4),
                 in1=sc_[:, 4:8].unsqueeze(2).to_broadcast([128, 4, 64]), op=ALU.mult, r=qkr + [ksc], w=[("ml_ks", d, i2)])
            if not first:
                S.op("pool", "tensor_tensor", out=qs[d][i2][:].rearrange("p (h x) -> p h x", h=4), in0=qk[:, ti, 0:256].rearrange("p (h x) -> p h x", h=4),
                     in1=sc_[:, 0:4].unsqueeze(2).to_broadcast([128, 4, 64]), op=ALU.mult, r=qkr + [ksc], w=[("ml_qs", d, i2)])
                bt = 4
                pbf = g.ps[bt].bitcast(BF16)
                for j in range(2):
                    S.op("pe", "transpose", out=pbf[:, j * 128:(j + 1) * 128], in_=qs[d][i2][:, j * 128:(j + 1) * 128], identity=g.c["ident_b"][:],
                         r=[("ml_qs", d, i2), ("c", "ident_b")], w=[("ps", bt)])
                S.op("act", "activation", out=qsT[d][i2][:], in_=pbf[:, 0:256].rearrange("p (j t) -> p j t", j=2), func=AF.Copy, r=[("ps", bt)], w=[("ml_qsT", d, i2)])

        def post(idx, d):
            ti = orders[d][idx]
            first = idx == 0
            i2 = idx % 2
            sc_ = sc4[d][i2]
            ksc = ("ml_sc", d, i2)
            bc = 7
            for pr in range(2):
                S.op("pe", "matmul", out=g.ps[bc][:, pr * 130:(pr + 1) * 130], lhsT=ks[d][i2][:, pr * 128:(pr + 1) * 128],
                     rhs=vx[:, ti, 2 * pr:2 * pr + 2, :].rearrange("p h x -> p (h x)"), start=True, stop=True,
                     r=[("ml_ks", d, i2), ("ml_vx", ti), "ml_vx1"], w=[("ps", bc)])
            for h in range(4):
                pr, po = h // 2, (h % 2) * 64
                c0 = (h % 2) * 65
                S.op("dve", "scalar_tensor_tensor", out=Cf[d][po:po + 64, pr, c0:c0 + 65], in0=Cf[d][po:po + 64, pr, c0:c0 + 65],
                     scalar=sc_[po:po + 64, 12 + h:13 + h], in1=g.ps[bc][po:po + 64, pr * 130 + c0:pr * 130 + c0 + 65], op0=ALU.mult, op1=ALU.add,
                     r=[("ps", bc), ksc, ("ml_Cf", d)], w=[("ml_Cf", d)])
            S.op("act", "activation", out=Cb[d][(idx + 1) % 2][:], in_=Cf[d][:], func=AF.Copy, r=[("ml_Cf", d)], w=[("ml_Cb", d, (idx + 1) % 2)])
            bo = 5 + d
            for h in range(4):
                pr = h // 2
                S.op("pe", "matmul", out=g.ps[bo][:, h * 65:(h + 1) * 65], lhsT=Sm[d][i2][:, h, :], rhs=vx[:, ti, h, :], start=True, stop=first,
                     r=[("ml_Sm", d, i2), ("ml_vx", ti), "ml_vx1"], w=[("ps", bo)])
                if not first:
                    S.op("pe", "matmul", out=g.ps[bo][:, h * 65:(h + 1) * 65], lhsT=qsT[d][i2][:, pr, :],
                         rhs=Cb[d][i2][:, pr, (h % 2) * 65:(h % 2 + 1) * 65], start=False, stop=True, r=[("ml_qsT", d, i2), ("ml_Cb", d, i2)], w=[("ps", bo)])
            o3 = g.ps[bo][:, 0:260].rearrange("p (h x) -> p h x", x=65)
            S.op("act", "activation", out=rd[d][:], in_=o3[:, :, 64], func=AF.Abs, r=[("ps", bo)], w=[("ml_rd", d)])
            S.op("dve", "tensor_scalar", out=rd[d][:], in0=rd[d][:], scalar1=1.0, scalar2=None, op0=ALU.max, r=[("ml_rd", d)], w=[("ml_rd", d)])
            S.op("dve", "reciprocal", out=rd[d][:], in_=rd[d][:], r=[("ml_rd", d)], w=[("ml_rd", d)])
            S.op("dve", "tensor_tensor", out=hh[d][:, ti, :].rearrange("p (h x) -> p h x", h=4), in0=o3[:, :, 0:64],
                 in1=rd[d][:].unsqueeze(2).to_broadcast([128, 4, 64]), op=ALU.mult, r=[("ps", bo), ("ml_rd", d)], w=[("ml_h", d, ti)])

        for d in range(2):
            pre(0, d)
        for idx in range(NT):
            if idx + 1 < NT:
                for d in range(2):
                    pre(idx + 1, d)
            for d in range(2):
                post(idx, d)
        hs = [g.sb(f"ml_hs{i}", [128, 256], F32, sc) for i in range(2)]
        hq = [g.sb(f"ml_hq{i}", [128, 256], F32, sc) for i in range(2)]
        m4 = [g.sb(f"ml_m4{i}", [128, 4], F32, sc) for i in range(2)]
        for ti in range(0 if need_ctx else 2, NT):
            i = ti % 2
            S.op("dve", "tensor_tensor", out=hs[i][:], in0=hh[0][:, ti, :], in1=hh[1][:, ti, :], op=ALU.add, r=[("ml_h", 0, ti), ("ml_h", 1, ti)], w=[("ml_hs", i)])
            S.op("act", "activation", out=hq[i][:], in_=hs[i][:], func=AF.Square, r=[("ml_hs", i)], w=[("ml_hq", i)])
            S.op("dve", "tensor_reduce", out=m4[i][:], in_=hq[i][:].rearrange("p (h x) -> p h x", h=4), axis=AX.X, op=ALU.add, r=[("ml_hq", i)], w=[("ml_m4", i)])
            S.op("dve", "tensor_scalar", out=m4[i][:], in0=m4[i][:], scalar1=1.0 / 64, scalar2=EPS, op0=ALU.mult, op1=ALU.add, r=[("ml_m4", i)], w=[("ml_m4", i)])
            rsqrt_inplace(g, m4[i][:], [("ml_m4", i)])
            S.op("dve", "tensor_tensor", out=hs[i][:].rearrange("p (h x) -> p h x", h=4), in0=hs[i][:].rearrange("p (h x) -> p h x", h=4),
                 in1=m4[i][:].unsqueeze(2).to_broadcast([128, 4, 64]), op=ALU.mult, r=[("ml_hs", i), ("ml_m4", i)], w=[("ml_hs", i)])
            S.op("pool", "tensor_tensor", out=hs[i][:], in0=hs[i][:], in1=nwb[:], op=ALU.mult, r=[("ml_hs", i), "ml_nwb"], w=[("ml_hs", i)])
            S.op("pool", "tensor_tensor", out=hs[i][:], in0=hs[i][:], in1=osig[:, ti, :], op=ALU.mult, r=[("ml_hs", i), ("ml_osig", ti)], w=[("ml_hs", i)])
            S.dma("sp", g.mix_d[b][ti * 128:(ti + 1) * 128, 0:256], hs[i][:], r=[("ml_hs", i)], w=[("d", "mix", b)])


def phase_ffn(g, l, b, need_ctx):
    S = g.S
    ne = g.ne
    last = (l == DEPTH - 1)
    groups = GROUPS if need_ctx else GROUPS[1:]
    with ExitStack() as sc:
        xT = g.sb("ff_xT", [128, 8, T], F32, sc)
        hx = g.sb("ff_hx", [128, 8, T], BF16, sc)
        xsrc = g.xT_d[b].rearrange("(c p) t -> p c t", p=128)
        for gi, (t0, W, j) in enumerate(groups):
            S.dma("sp", xT[:, :, t0:t0 + W], xsrc[:, :, t0:t0 + W], r=[("d", "xT", b)], w=[("ff_xT", t0)])
        with ExitStack() as s2:
            wout = g.sb("ff_wout", [128, 8, D], BF16, s2)
            gateT = g.sb("ff_gateT", [32, T], F32, s2)
            b2s = g.sb("mo_b2", [32, D], F32, s2)
            rw = g.sb("ff_rw", [128, 8, NE], F32, s2)
            rb = g.sb("ff_rb", [128, NE], F32, s2)
            mt = [g.sb(f"ff_mt{i}", [128, D], F32, s2) for i in range(2)]
            mtb = [g.sb(f"ff_mtb{i}", [128, D], BF16, s2) for i in range(2)]
            mixT = [g.sb(f"ff_mixT{i}", [128, 8, 512], BF16, s2) for i in range(2)]
            sq = g.sb("ff_sq", [128, 8, 512], BF16, s2)
            rstd = g.sb("ff_rstd", [128, 512], F32, s2)
            tmp = [g.sb(f"ff_tmp{i}", [128, 512], F32, s2) for i in range(2)]
            hxf = g.sb("ff_hxf", [128, 8, 512], F32, s2)
            lg = [g.sb(f"ff_lg{i}", [128, NE], F32, s2) for i in range(2)]
            t8 = [g.sb(f"ff_t8{i}", [128, 8], F32, s2) for i in range(2)]
            ms = [g.sb(f"ff_ms{i}", [128, NE], F32, s2) for i in range(2)]
            s1 = [g.sb(f"ff_s1{i}", [128, 2], F32, s2) for i in range(2)]
            S.dma("pool", wout[:], g.inp["w_out"][l].rearrange("(c p) f -> p c f", p=128), w=["ff_wout"])
            S.dma("sp", rw[:], g.inp["router_w"][l].rearrange("(c p) f -> p c f", p=128), w=["ff_rw"])
            S.dma("sp", rb[:], g.inp["router_b"][l].partition_broadcast(128), w=["ff_rb"])
            stt = dict(nbk=0, ntile=0)

            def load_mix(gi):
                t0, W, j = groups[gi]
                ms_ = gi % 2
                for tt in range(W // 128):
                    i = stt["ntile"] % 2
                    stt["ntile"] += 1
                    S.dma("sp", mt[i][:], g.mix_d[b][t0 + tt * 128:t0 + (tt + 1) * 128, :], r=[("d", "mix", b)], w=[("ff_mt", i)])
                    S.op("act", "activation", out=mtb[i][:], in_=mt[i][:], func=AF.Copy, r=[("ff_mt", i)], w=[("ff_mtb", i)])
                    bk = stt["nbk"] % 8
                    stt["nbk"] += 1
                    pbf = g.ps[bk].bitcast(BF16)
                    for c in range(8):
                        S.op("pe", "transpose", out=pbf[:, c * 128:(c + 1) * 128], in_=mtb[i][:, c * 128:(c + 1) * 128], identity=g.c["ident_b"][:],
                             r=[("ff_mtb", i), ("c", "ident_b")], w=[("ps", bk)])
                    evac(g, tt, mixT[ms_][:, :, tt * 128:(tt + 1) * 128], pbf[:, :].rearrange("p (c t) -> p c t", c=8), r=[("ps", bk)], w=[("ff_mixT", ms_, tt)])

            def ffn_group(gi):
                t0, W, j = groups[gi]
                ms_ = gi % 2
                xk = ("ff_xT", t0)
                nbk = stt["nbk"]
                mr = [("ff_mixT", ms_, tt) for tt in range(W // 128)]
                for dc in range(8):
                    bk = nbk % 8
                    nbk += 1
                    for fc in range(8):
                        S.op("pe", "matmul", out=g.ps[bk][:, 0:W], lhsT=wout[:, fc, dc * 128:(dc + 1) * 128], rhs=mixT[ms_][:, fc, 0:W], start=(fc == 0), stop=(fc == 7),
                             r=mr + ["ff_wout"], w=[("ps", bk)])
                    S.op("dve", "scalar_tensor_tensor", out=xT[:, dc, t0:t0 + W], in0=g.ps[bk][:, 0:W], scalar=g.modT[:, 16 + dc, j:j + 1], in1=xT[:, dc, t0:t0 + W],
                         op0=ALU.mult, op1=ALU.add, r=[("ps", bk), "modT", xk], w=[xk])
                S.op("act", "activation", out=sq[:, :, 0:W], in_=xT[:, :, t0:t0 + W], func=AF.Square, r=[xk], w=["ff_sq"])
                bk = nbk % 8
                nbk += 1
                for kc in range(8):
                    S.op("pe", "matmul", out=g.ps[bk][:, 0:W], lhsT=g.c["ones_b"][:], rhs=sq[:, kc, 0:W], start=(kc == 0), stop=(kc == 7),
                         r=["ff_sq", ("c", "ones_b")], w=[("ps", bk)])
                S.op("dve", "tensor_scalar", out=rstd[:, 0:W], in0=g.ps[bk][:, 0:W], scalar1=1.0 / D, scalar2=EPS, op0=ALU.mult, op1=ALU.add,
                     r=[("ps", bk)], w=["ff_rstd"])
                rsqrt_inplace(g, rstd[:, 0:W], ["ff_rstd"])
                for kc in range(8):
                    ts = kc % 2
                    S.op("dve", "tensor_tensor", out=tmp[ts][:, 0:W], in0=xT[:, kc, t0:t0 + W], in1=rstd[:, 0:W], op=ALU.mult, r=[xk, "ff_rstd"], w=[("ff_tmp", ts)])
                    S.op("act", "activation", out=hxf[:, kc, 0:W], in_=tmp[ts][:, 0:W], func=AF.Identity, scale=g.a2[:, kc, j:j + 1], bias=g.modT[:, 24 + kc, j:j + 1],
                         r=[("ff_tmp", ts), ("a", 1), "modT"], w=[("ff_hxf", kc)])
                    S.op("pool", "tensor_copy", out=hx[:, kc, t0:t0 + W], in_=hxf[:, kc, 0:W], r=[("ff_hxf", kc)], w=[("ff_hx", t0)])
                hr = [("ff_hxf", kc) for kc in range(8)]
                for tt in range(W // 128):
                    i = tt % 2
                    bk = nbk % 8
                    nbk += 1
                    for kc in range(8):
                        S.op("pe", "matmul", out=g.ps[bk][:, 0:NE], lhsT=hxf[:, kc, tt * 128:(tt + 1) * 128], rhs=rw[:, kc, :], start=(kc == 0), stop=(kc == 7),
                             r=hr + ["ff_rw"], w=[("ps", bk)])
                    kl, k8, km, ks1 = ("ff_lg", i), ("ff_t8", i), ("ff_ms", i), ("ff_s1", i)
                    S.op("dve", "tensor_tensor", out=lg[i][:], in0=g.ps[bk][:, 0:NE], in1=rb[:], op=ALU.add, r=[("ps", bk), "ff_rb"], w=[kl])
                    S.op("dve", "max", out=t8[i][:], in_=lg[i][:], r=[kl], w=[k8])
                    S.op("dve", "tensor_scalar", out=ms[i][:], in0=lg[i][:], scalar1=t8[i][:, 3:4], scalar2=None, op0=ALU.is_ge, r=[kl, k8], w=[km])
                    S.op("dve", "tensor_scalar", out=s1[i][:, 0:1], in0=t8[i][:, 0:1], scalar1=-1.0, scalar2=None, op0=ALU.mult, r=[k8], w=[ks1])
                    S.op("act", "activation", out=lg[i][:], in_=lg[i][:], func=AF.Exp, bias=s1[i][:, 0:1], r=[kl, ks1], w=[kl])
                    S.op("dve", "tensor_tensor", out=lg[i][:], in0=lg[i][:], in1=ms[i][:], op=ALU.mult, r=[kl, km], w=[kl])
                    S.op("dve", "tensor_reduce", out=s1[i][:, 1:2], in_=lg[i][:], axis=AX.X, op=ALU.add, r=[kl, ks1], w=[ks1])
                    S.op("dve", "reciprocal", out=s1[i][:, 1:2], in_=s1[i][:, 1:2], r=[ks1], w=[ks1])
                    S.op("dve", "tensor_scalar", out=lg[i][:], in0=lg[i][:], scalar1=s1[i][:, 1:2], scalar2=None, op0=ALU.mult, r=[kl, ks1], w=[kl])
                    bk2 = nbk % 8
                    nbk += 1
                    S.op("pe", "transpose", out=g.ps[bk2][0:NE, 0:128], in_=lg[i][:], identity=g.c["ident_f"][:], r=[kl, ("c", "ident_f")], w=[("ps", bk2)])
                    S.op("act", "activation", out=gateT[:, t0 + tt * 128:t0 + (tt + 1) * 128], in_=g.ps[bk2][0:NE, 0:128], func=AF.Copy, r=[("ps", bk2)], w=[("ff_gateT", t0)])
                stt["nbk"] = nbk

            load_mix(0)
            for gi in range(len(groups)):
                if gi + 1 < len(groups):
                    load_mix(gi + 1)
                ffn_group(gi)
            nbk = stt["nbk"]
            S.dma("sp", b2s[:], g.inp["exp_b2"][l], w=["mo_b2"])
            for (t0, W, j) in groups:
                for dc in range(8):
                    bk = nbk % 8
                    nbk += 1
                    S.op("pe", "matmul", out=g.ps[bk][:, 0:W], lhsT=b2s[0:32, dc * 128:(dc + 1) * 128], rhs=gateT[0:32, t0:t0 + W], start=True, stop=True,
                         r=["mo_b2", ("ff_gateT", t0)], w=[("ps", bk)])
                    S.op("dve", "scalar_tensor_tensor", out=xT[:, dc, t0:t0 + W], in0=g.ps[bk][:, 0:W], scalar=g.modT[:, 40 + dc, j:j + 1], in1=xT[:, dc, t0:t0 + W],
                         op0=ALU.mult, op1=ALU.add, r=[("ps", bk), "modT", ("ff_xT", t0)], w=[("ff_xT", t0)])
            for (t0, W, j) in groups:
                S.dma("sp", g.gate_d[b][:, t0:t0 + W], gateT[:, t0:t0 + W], r=[("ff_gateT", t0)], w=[("d", "gate", b)])
        S.barrier()
        with ExitStack() as s3:
            w1 = [g.sb(f"mo_w1{i}", [128, 8, 2, 512], BF16, s3) for i in range(2)]
            w2 = [g.sb(f"mo_w2{i}", [128, 4, D], BF16, s3) for i in range(2)]
            b1T = g.sb("mo_b1T", [128, NE, 16], F32, s3)
            gbc = [g.sb(f"mo_gbc{i}", [128, T], F32, s3) for i in range(2)]
            gl_ = [g.sb(f"mo_gl{i}", [128, 512], F32, s3) for i in range(2)]
            sg_ = [g.sb(f"mo_sg{i}", [128, 512], F32, s3) for i in range(2)]
            ln_ = [g.sb(f"mo_ln{i}", [128, 512], F32, s3) for i in range(2)]
            act = [g.sb(f"mo_act{i}", [128, 4, 512], BF16, s3) for i in range(2)]
            for e0 in range(0, NE, 4):
                S.dma("sp", b1T[:, e0:e0 + 4, :], g.inp["exp_b1"][l, e0:e0 + 4, :].rearrange("e (c p) -> p e c", p=128), w=["mo_b1T"])
            S.op("dve", "tensor_scalar", out=b1T[:, :, 8:16], in0=b1T[:, :, 8:16], scalar1=1.0, scalar2=None, op0=ALU.add, r=["mo_b1T"], w=["mo_b1T"])
            nbk = 0
            steps = [(e, hf) for e in range(ne) for hf in range(2)]
            units = [(k, gi) for k in range(len(steps)) for gi in range(len(groups))]
            t00 = groups[0][0]

            def load_w(k):
                e, hf = steps[k]
                wi = k % 2
                if hf == 0:
                    S.dma("sp", gbc[e % 2][:, t00:T], g.gate_d[b][e, t00:T].partition_broadcast(128), r=[("d", "gate", b)], w=[("mo_gbc", e % 2)])
                w1src = g.inp["exp_w1"][l, e].rearrange("(c p) f -> p c f", p=128)
                for q2 in range(2):
                    S.dma("pool", w1[wi][:, :, q2, :], w1src[:, :, q2 * 1024 + hf * 512:q2 * 1024 + (hf + 1) * 512], w=[("mo_w1", wi, q2)])
                S.dma("pool", w2[wi][:], g.inp["exp_w2"][l, e, hf * 512:(hf + 1) * 512, :].rearrange("(c p) d -> p c d", p=128), w=[("mo_w2", wi)])

            def W1(u, jp):
                k, gi = units[u]
                e, hf = steps[k]
                wi, ei, ai = k % 2, e % 2, u % 2
                t0, W, j = groups[gi]
                pbanks = {}
                for jc in (2 * jp, 2 * jp + 1):
                    for q2 in range(2):
                        bq = (jc % 2) * 2 + q2
                        pbanks[(jc, q2)] = bq
                        for kc in range(8):
                            S.op("pe", "matmul", out=g.ps[bq][:, 0:W], lhsT=w1[wi][:, kc, q2, jc * 128:(jc + 1) * 128], rhs=hx[:, kc, t0:t0 + W],
                                 start=(kc == 0), stop=(kc == 7), r=[("mo_w1", wi, q2), ("ff_hx", t0)], w=[("ps", bq)])
                pr_ = [(jc, jc % 2, hf * 4 + jc) for jc in (2 * jp, 2 * jp + 1)]
                for jc, ii, fidx in pr_:
                    S.op("dve", "tensor_scalar", out=gl_[ii][:, 0:W], in0=g.ps[pbanks[(jc, 0)]][:, 0:W], scalar1=b1T[:, e, fidx:fidx + 1], scalar2=7.0,
                         op0=ALU.add, op1=ALU.min, r=[("ps", pbanks[(jc, 0)]), "mo_b1T"], w=[("mo_gl", ii)])
                for jc, ii, fidx in pr_:
                    S.op("act", "activation", out=sg_[ii][:, 0:W], in_=gl_[ii][:, 0:W], func=AF.Sigmoid, scale=1.702, r=[("mo_gl", ii)], w=[("mo_sg", ii)])
                for jc, ii, fidx in pr_:
                    S.op("dve", "tensor_scalar", out=ln_[ii][:, 0:W], in0=g.ps[pbanks[(jc, 1)]][:, 0:W], scalar1=b1T[:, e, 8 + fidx:9 + fidx], scalar2=8.0,
                         op0=ALU.add, op1=ALU.min, r=[("ps", pbanks[(jc, 1)]), "mo_b1T"], w=[("mo_ln", ii)])
                for jc, ii, fidx in pr_:
                    S.op("dve", "tensor_tensor", out=gl_[ii][:, 0:W], in0=gl_[ii][:, 0:W], in1=sg_[ii][:, 0:W], op=ALU.mult,
                         r=[("mo_gl", ii), ("mo_sg", ii)], w=[("mo_gl", ii)])
                for jc, ii, fidx in pr_:
                    S.op("dve", "scalar_tensor_tensor", out=gl_[ii][:, 0:W], in0=ln_[ii][:, 0:W], scalar=-6.0, in1=gl_[ii][:, 0:W], op0=ALU.max, op1=ALU.mult,
                         r=[("mo_gl", ii), ("mo_ln", ii)], w=[("mo_gl", ii)])
                for jc, ii, fidx in pr_:
                    S.op("pool", "tensor_tensor", out=act[ai][:, jc, 0:W], in0=gl_[ii][:, 0:W], in1=gbc[ei][:, t0:t0 + W], op=ALU.mult,
                         r=[("mo_gl", ii), ("mo_gbc", ei)], w=[("mo_act", ai, jc)])

            w2cnt = [0]

            def W2(u, dcs):
                k, gi = units[u]
                wi, ai = k % 2, u % 2
                t0, W, j = groups[gi]
                ar = [("mo_act", ai, jc) for jc in range(4)]
                for dc in dcs:
                    bk = 4 + w2cnt[0] % 4
                    w2cnt[0] += 1
                    for jc in range(4):
                        S.op("pe", "matmul", out=g.ps[bk][:, 0:W], lhsT=w2[wi][:, jc, dc * 128:(dc + 1) * 128], rhs=act[ai][:, jc, 0:W], start=(jc == 0), stop=(jc == 3),
                             r=ar + [("mo_w2", wi)], w=[("ps", bk)])
                    S.op("dve", "scalar_tensor_tensor", out=xT[:, dc, t0:t0 + W], in0=g.ps[bk][:, 0:W], scalar=g.modT[:, 40 + dc, j:j + 1], in1=xT[:, dc, t0:t0 + W],
                         op0=ALU.mult, op1=ALU.add, r=[("ps", bk), "modT", ("ff_xT", t0)], w=[("ff_xT", t0)])

            load_w(0)
            W1(0, 0)
            W1(0, 1)
            for u in range(len(units)):
                k, gi = units[u]
                if gi == 0 and k + 1 < len(steps):
                    load_w(k + 1)
                nxt = u + 1 < len(units)
                if nxt:
                    W1(u + 1, 0)
                W2(u, range(0, 4))
                if nxt:
                    W1(u + 1, 1)
                W2(u, range(4, 8))
        S.barrier()
        if not last:
            for (t0, W, j) in groups:
                S.dma("sp", xsrc[:, :, t0:t0 + W], xT[:, :, t0:t0 + W], r=[("ff_xT", t0)], w=[("d", "xT", b)])
        else:
            with ExitStack() as s4:
                ot = [g.sb(f"fo_t{i}", [128, D], F32, s4) for i in range(2)]
                nbk = 0
                for ti in range(2, NT):
                    i = ti % 2
                    t0g = 256 + ((ti * 128 - 256) // 512) * 512
                    for h2 in range(2):
                        bk = nbk % 8
                        nbk += 1
                        for c4 in range(4):
                            c = h2 * 4 + c4
                            S.op("pe", "transpose", out=g.ps[bk][:, c4 * 128:(c4 + 1) * 128], in_=xT[:, c, ti * 128:(ti + 1) * 128], identity=g.c["ident_f"][:],
                                 r=[("ff_xT", t0g), ("c", "ident_f")], w=[("ps", bk)])
                        evac(g, h2, ot[i][:, h2 * 512:(h2 + 1) * 512], g.ps[bk][:, :], r=[("ps", bk)], w=[("fo_t", i, h2)])
                    S.dma("sp", g.y[b, (ti - 2) * 128:(ti - 1) * 128, :], ot[i][:], r=[("fo_t", i, 0), ("fo_t", i, 1)], w=[("d", "y")])


_CONSTS = None


def make_in_map(inp, core):
    global _CONSTS
    if _CONSTS is None:
        _CONSTS = _consts()
    b0 = 2 * core
    m = dict(_CONSTS)
    f = lambda a: np.ascontiguousarray(np.asarray(a, dtype=np.float32))
    m["x"] = f(inp["x"][b0:b0 + 2])
    m["ctx"] = f(inp["ctx"][b0:b0 + 2])
    cc = np.asarray(inp["c_ctx"], np.float32)
    m["cs"] = f(np.stack([np.stack([np.asarray(inp["c"][b0 + i], np.float32), cc]) for i in range(2)]))
    for k in ("norm_mix_w", "norm_ffn_w", "w_ada", "b_ada", "w_in", "mlstm_norm_w", "na_qnorm_w", "na_knorm_w", "conv_w", "conv_b",
              "conv_ln_w", "conv_ln_b", "w_out", "router_w", "router_b", "exp_w1", "exp_b1", "exp_w2", "exp_b2"):
        m[k] = f(inp[k])
    m["mlstm_ig_b"] = f(np.asarray(inp["mlstm_ig_b"]).reshape(DEPTH, 8))
    m["mlstm_fg_b"] = f(np.asarray(inp["mlstm_fg_b"]).reshape(DEPTH, 8))
    col = np.arange(64)
    dc = np.clip(col[:, None] - col[None, :] + 15, 0, 30)
    rp = np.asarray(inp["na_rpb"], np.float32)[:, :, :, dc]
    m["rpbT"] = f(rp.reshape(DEPTH, 8, 15 * 64, 64))
    return m


_NC_CACHE = {}


def kernel(**inputs):
    if "nc" not in _NC_CACHE:
        _NC_CACHE["nc"] = build_program()
    nc = _NC_CACHE["nc"]
    in_maps = [make_in_map(inputs, c) for c in range(8)]
    res = run_bass_kernel_spmd(nc, in_maps, core_ids=list(range(8)))
    out = np.concatenate([np.asarray(r["y"], dtype=np.float32) for r in res.results], axis=0)
    return out
```

```python
from contextlib import ExitStack

import ml_dtypes
import numpy as np

import concourse.bass as bass
import concourse.mybir as mybir
from concourse.bass_utils import run_bass_kernel_spmd

F32 = mybir.dt.float32
BF16 = mybir.dt.bfloat16
AF = mybir.ActivationFunctionType
ALU = mybir.AluOpType
AX = mybir.AxisListType

D = 1024
SEQ = 2048
CTX = 256
T = SEQ + CTX
NT = T // 128
DEPTH = 2
NE = 32
IN_COLS = 3088
TOKC = 2576
A_Q, A_K, A_V, A_O, A_G = 0, 256, 512, 768, 1024
B_Q, B_K, B_V = 1040, 1552, 2064
EPS = 1e-6
NEG = -30000.0
GROUPS = [(0, 256, 1), (256, 512, 0), (768, 512, 0), (1280, 512, 0), (1792, 512, 0)]


class Sched:
    CE = ("pe", "dve", "act", "pool")
    NSLOT = 8

    def __init__(self, nc, es):
        self.nc = nc
        self.es = es
        self.eng = {"pe": nc.tensor, "dve": nc.vector, "act": nc.scalar, "pool": nc.gpsimd, "sp": nc.sync}
        self.ops = []
        self.last_w = {}
        self.readers = {}
        self.epoch = 0

    def _rec(self, kind, eng, fn, r, w, kw):
        idx = len(self.ops)
        deps = set()
        for k in r:
            p = self.last_w.get(k)
            if p is not None:
                deps.add(p)
        for k in w:
            p = self.last_w.get(k)
            if p is not None:
                deps.add(p)
            deps.update(self.readers.get(k, ()))
        for k in r:
            lst = self.readers.setdefault(k, [])
            if kind == "c":
                lst[:] = [j for j in lst if not (self.ops[j]["kind"] == "c" and self.ops[j]["eng"] == eng)]
            lst.append(idx)
        for k in w:
            self.last_w[k] = idx
            self.readers[k] = []
        deps.discard(idx)
        op = dict(kind=kind, eng=eng, fn=fn, kw=kw, deps=deps, sig=False, epoch=self.epoch)
        for d in deps:
            po = self.ops[d]
            if po["epoch"] == self.epoch and not (po["eng"] == "pe" and eng == "pe" and po["kind"] == "c" and kind == "c"):
                po["sig"] = True
        self.ops.append(op)
        return idx

    def op(self, eng, fn, r=(), w=(), **kw):
        return self._rec("c", eng, fn, r, w, kw)

    def dma(self, q, out, in_, r=(), w=()):
        return self._rec("d", q, "dma_start", r, w, dict(out=out, in_=in_))

    def barrier(self):
        self.ops.append(dict(kind="b", epoch=self.epoch))
        self.epoch += 1

    def emit(self):
        nc = self.nc
        sems = None
        cnt = dcnt = waited = dwaited = None
        slots = {q: [self.es.enter_context(nc.semaphore(f"dq_{q}{i}")) for i in range(self.NSLOT)] for q in ("sp", "pool")}
        dissued = {"sp": 0, "pool": 0}
        allq = self.CE + ("sp",)

        sems = {e: self.es.enter_context(nc.semaphore(f"s_{e}")) for e in self.CE}
        cnt = {e: 0 for e in self.CE}
        waited = {q: {e: 0 for e in self.CE} for q in allq}
        dwaited = {q: {} for q in allq}

        def new_epoch(ep):
            pass

        new_epoch(0)
        last = {}
        for i, o in enumerate(self.ops):
            if o["kind"] == "b":
                for e, j in last.items():
                    self.ops[j]["sig"] = True
                last = {}
            elif o["kind"] == "c":
                last[o["eng"]] = i
        for e, j in last.items():
            self.ops[j]["sig"] = True

        TR = False

        def wait_dma(q, key, val):
            if dwaited[q].get(key, 0) < val:
                self.eng[q].wait_ge(slots[key[0]][key[1]], val)
                dwaited[q][key] = val
                if TR:
                    print(f"[{q}] wait dma{key} >= {val}")

        def wait_eng(q, e, val):
            if waited[q][e] < val:
                self.eng[q].wait_ge(sems[e], val)
                waited[q][e] = val
                if TR:
                    print(f"[{q}] wait {e} >= {val}")

        ep = 0
        for o in self.ops:
            if o["kind"] == "b":
                for q in allq:
                    for e in self.CE:
                        wait_eng(q, e, cnt[e])
                    for dq in ("sp", "pool"):
                        n = dissued[dq]
                        for s in range(self.NSLOT):
                            nuse = (n - s + self.NSLOT - 1) // self.NSLOT if n > s else 0
                            if nuse:
                                wait_dma(q, (dq, s), 16 * nuse)
                ep += 1
                new_epoch(ep)
                continue
            q = o["eng"]
            for d in sorted(o["deps"]):
                p = self.ops[d]
                if p["epoch"] != ep:
                    continue
                if p["kind"] == "d":
                    wait_dma(q, p["dkey"], p["dval"])
                elif p["sig"]:
                    wait_eng(q, p["eng"], p["sigval"])
            if o["kind"] == "d":
                n = dissued[q]
                s = n % self.NSLOT
                use = n // self.NSLOT
                if use > 0:
                    wait_dma(q, (q, s), 16 * use)
                ins = self.eng[q].dma_start(**o["kw"])
                ins.then_inc(slots[q][s], 16)
                o["dkey"] = (q, s)
                o["dval"] = 16 * (use + 1)
                dissued[q] = n + 1
                if TR:
                    print(f"[{q}] DMA -> dma{(q, s)} = {o['dval']}  out={o['kw']['out'].tensor.name}")
            else:
                ins = getattr(self.eng[q], o["fn"])(**o["kw"])
                if o["sig"]:
                    ins.then_inc(sems[q], 1)
                    cnt[q] += 1
                    o["sigval"] = cnt[q]
                if TR:
                    print(f"[{q}] {o['fn']} sig={o.get('sigval')} out={[v.tensor.name for k, v in o['kw'].items() if k in ('out', 'ap')]}")
        for q in allq:
            for e in self.CE:
                wait_eng(q, e, cnt[e])
            for dq in ("sp", "pool"):
                n = dissued[dq]
                for s in range(self.NSLOT):
                    nuse = (n - s + self.NSLOT - 1) // self.NSLOT if n > s else 0
                    if nuse:
                        wait_dma(q, (dq, s), 16 * nuse)


def _consts():
    c = {}
    c["ident_f"] = np.eye(128, dtype=np.float32)
    c["ident_b"] = np.eye(128, dtype=np.float32).astype(ml_dtypes.bfloat16)
    c["ones_b"] = np.ones((128, 128), np.float32).astype(ml_dtypes.bfloat16)
    c["ones_f"] = np.ones((128, 128), np.float32)
    u = np.arange(128)
    triu = (u[:, None] <= u[None, :]).astype(np.float32)
    tril = (u[:, None] >= u[None, :]).astype(np.float32)
    c["tri"] = np.stack([triu] * 4 + [tril] * 4, axis=1).astype(np.float32)
    nm = np.zeros((128, 2, 4, 128), np.float32)
    nm[:, 0, :, :] = np.where(u[:, None] > u[None, :], NEG, 0.0)[:, None, :]
    nm[:, 1, :, :] = np.where(u[:, None] < u[None, :], NEG, 0.0)[:, None, :]
    c["negmask"] = nm.reshape(128, 2, 512)
    quarter = 16
    inv = (10000.0 ** (-np.arange(quarter, dtype=np.float32) / quarter)).astype(np.float32)
    t = np.arange(SEQ)
    rows = (t // 64).astype(np.float32)
    cols = (t % 64).astype(np.float32)
    ang = np.stack([rows[:, None] * inv[None, :], cols[:, None] * inv[None, :]], axis=1).astype(np.float32)
    cs, sn = np.cos(ang).astype(np.float32), np.sin(ang).astype(np.float32)
    C = np.ones((T, 2, 4, 2, 16), np.float32)
    Sn = np.zeros((T, 2, 4, 2, 16), np.float32)
    C[CTX:] = cs[:, None, None, :, :]
    Sn[CTX:] = sn[:, None, None, :, :]
    C[:, 1] *= 0.125
    Sn[:, 1] *= 0.125
    c["ropeC"] = C.reshape(T, 256)
    c["ropeS"] = Sn.reshape(T, 256)
    col = np.arange(64)
    cstart = np.clip(col - 8, 0, 48)
    inw = (col[None, :] >= cstart[:, None]) & (col[None, :] < cstart[:, None] + 16)
    cm = inw.T.astype(np.float32)
    c["cmask"] = np.concatenate([cm, cm], axis=0)
    return c


CONST_SPECS = [("ident_f", [128, 128], F32), ("ident_b", [128, 128], BF16), ("ones_b", [128, 128], BF16),
               ("ones_f", [128, 128], F32), ("tri", [128, 8, 128], F32), ("negmask", [128, 2, 512], F32),
               ("ropeC", [T, 256], F32), ("ropeS", [T, 256], F32), ("cmask", [128, 64], F32)]

IN_SPECS = [("x", [2, SEQ, D]), ("ctx", [2, CTX, D]), ("cs", [2, 2, D]),
            ("norm_mix_w", [DEPTH, D]), ("norm_ffn_w", [DEPTH, D]), ("w_ada", [DEPTH, D, 6 * D]), ("b_ada", [DEPTH, 6 * D]),
            ("w_in", [DEPTH, D, IN_COLS]), ("mlstm_ig_b", [DEPTH, 8]), ("mlstm_fg_b", [DEPTH, 8]),
            ("mlstm_norm_w", [DEPTH, 256]), ("na_qnorm_w", [DEPTH, 64]), ("na_knorm_w", [DEPTH, 64]),
            ("rpbT", [DEPTH, 8, 15 * 64, 64]), ("conv_w", [DEPTH, 31, 256]), ("conv_b", [DEPTH, 256]),
            ("conv_ln_w", [DEPTH, 256]), ("conv_ln_b", [DEPTH, 256]), ("w_out", [DEPTH, D, D]),
            ("router_w", [DEPTH, D, NE]), ("router_b", [DEPTH, NE]), ("exp_w1", [DEPTH, NE, D, 2 * D]),
            ("exp_b1", [DEPTH, NE, 2 * D]), ("exp_w2", [DEPTH, NE, D, D]), ("exp_b2", [DEPTH, NE, D])]


class Ctx:
    pass


def build_program(stop_after=None, n_batch=2, depth=DEPTH, dbg=None, ne=NE):
    nc = bass.Bass("TRN2", target_bir_lowering=False)
    es = ExitStack()
    S = Sched(nc, es)
    g = Ctx()
    g.nc, g.S, g.ne = nc, S, ne
    g.inp = {n: nc.dram_tensor(n, s, F32, kind="ExternalInput") for n, s in IN_SPECS}
    g.cin = {n: nc.dram_tensor(n, s, dt, kind="ExternalInput") for n, s, dt in CONST_SPECS}
    g.y = nc.dram_tensor("y", [2, SEQ, D], F32, kind="ExternalOutput")
    g.dbg = {}
    for n, s in (dbg or []):
        g.dbg[n] = nc.dram_tensor(n, s, F32, kind="ExternalOutput")
    g.xT_d = [nc.dram_tensor(f"xT_d{b}", [D, T], F32) for b in range(2)]
    g.ptok_d = [nc.dram_tensor(f"ptok_d{b}", [T, TOKC], F32) for b in range(2)]
    g.pcT_d = [nc.dram_tensor(f"pcT_d{b}", [512, T], F32) for b in range(2)]
    g.mix_d = [nc.dram_tensor(f"mix_d{b}", [T, D], F32) for b in range(2)]
    g.gate_d = [nc.dram_tensor(f"gate_d{b}", [32, T], F32) for b in range(2)]

    uid = [0]

    def sb(name, shape, dt=F32, scope=None):
        uid[0] += 1
        return (scope or es).enter_context(nc.sbuf_tensor(f"{name}_u{uid[0]}", shape, dt))

    g.sb = sb
    g.c = {}
    for n, s, dt in CONST_SPECS:
        if n in ("ropeC", "ropeS", "tri", "negmask", "cmask"):
            continue
        g.c[n] = sb("c_" + n, s, dt)
        S.dma("sp", g.c[n][:], g.cin[n][:], w=[("c", n)])
    g.ps = [es.enter_context(nc.psum_tensor(f"ps{i}", [128, 512], F32)) for i in range(8)]
    g.modT = sb("modT", [128, 48, 2])
    g.a1 = sb("a1", [128, 8, 2])
    g.a2 = sb("a2", [128, 8, 2])
    S.barrier()

    done = False
    for b in range(n_batch):
        phase_start(g, b)
        S.barrier()
        for l in range(depth):
            need_ctx = l < DEPTH - 1
            for ph in (phase_ada, phase_proj, phase_na, phase_mlstm, phase_conv, phase_ffn):
                if ph is phase_ada:
                    pre = ExitStack()
                    g.win = sb("pj_win", [128, 8, IN_COLS], BF16, pre)
                    wsrc = g.inp["w_in"][l].rearrange("(c p) f -> p c f", p=128)
                    for i, (c0, c1) in enumerate(((0, 772), (772, 1544), (1544, 2316), (2316, IN_COLS))):
                        S.dma("pool", g.win[:, :, c0:c1], wsrc[:, :, c0:c1], w=[("pj_win", i)])
                ph(g, l, b, need_ctx)
                S.barrier()
                if ph is phase_proj:
                    pre.close()
                if stop_after == (ph.__name__, l, b):
                    done = True
                    break
            if done:
                break
        if done:
            break
    if dbg:
        with ExitStack() as sc:
            buf = sb("dbgbuf", [128, 4096], F32, sc)
            for n, s in dbg:
                src = {"ptok": g.ptok_d[0], "pcT": g.pcT_d[0], "mix": g.mix_d[0], "xT": g.xT_d[0]}[n]
                rows, cols = s
                for r0 in range(0, rows, 128):
                    S.dma("sp", buf[:, 0:cols], src[r0:r0 + 128, :], r=[("d", src.name)], w=["dbgbuf"])
                    S.dma("sp", g.dbg[n][r0:r0 + 128, :], buf[:, 0:cols], r=["dbgbuf"], w=[("d", n)])
    with nc.allow_non_contiguous_dma(reason="small per-feature vector loads / layout changes"):
        S.emit()
    es.close()
    return nc


def rsqrt_inplace(g, ap, keys):
    g.S.op("act", "activation", out=ap, in_=ap, func=AF.Sqrt, r=keys, w=keys)
    g.S.op("dve", "reciprocal", out=ap, in_=ap, r=keys, w=keys)


def evac(g, i, out, in_, r, w):
    if i % 2 == 0:
        g.S.op("act", "activation", out=out, in_=in_, func=AF.Copy, r=r, w=w)
    else:
        g.S.op("dve", "tensor_copy", out=out, in_=in_, r=r, w=w)


def phase_start(g, b):
    S, nc = g.S, g.nc
    with ExitStack() as sc:
        xt = [g.sb(f"st_x{i}", [128, D], F32, sc) for i in range(2)]
        xo = [g.sb(f"st_o{i}", [128, 8, 128], F32, sc) for i in range(2)]
        dst = g.xT_d[b].rearrange("(c p) t -> p c t", p=128)
        for ti in range(NT):
            s = ti % 2
            src = g.inp["ctx"][b, ti * 128:(ti + 1) * 128, :] if ti < 2 else g.inp["x"][b, (ti - 2) * 128:(ti - 1) * 128, :]
            S.dma("sp", xt[s][:], src, w=[("st_x", s)])
            for h in range(2):
                pb = g.ps[(ti * 2 + h) % 8]
                for j in range(4):
                    cc = h * 4 + j
                    S.op("pe", "transpose", out=pb[:, j * 128:(j + 1) * 128], in_=xt[s][:, cc * 128:(cc + 1) * 128],
                         identity=g.c["ident_f"][:], r=[("st_x", s), ("c", "ident_f")], w=[("ps", (ti * 2 + h) % 8)])
                evac(g, h, xo[s][:, h * 4:(h + 1) * 4, :], pb[:, :].rearrange("p (j t) -> p j t", j=4),
                     r=[("ps", (ti * 2 + h) % 8)], w=[("st_o", s, h)])
            S.dma("sp", dst[:, :, ti * 128:(ti + 1) * 128], xo[s][:], r=[("st_o", s, 0), ("st_o", s, 1)], w=[("d", "xT", b)])


def load_vec(g, dst, src_1d, n, w, q="sp"):
    g.S.dma(q, dst, src_1d.rearrange("(c p o) -> p c o", p=128, o=1), w=w)


def phase_ada(g, l, b, need_ctx):
    S = g.S
    with ExitStack() as sc:
        scT = g.sb("ada_sc", [128, 8, 2], F32, sc)
        brow = g.sb("ada_brow", [2, 6 * D], F32, sc)
        mrow = g.sb("ada_mrow", [2, 6 * D], F32, sc)
        nw = g.sb("ada_nw", [128, 8, 2], F32, sc)
        tmp = g.sb("ada_tmp", [128, 8, 2], F32, sc)
        wa = [g.sb(f"ada_w{i}", [128, 8, 512], F32, sc) for i in range(2)]
        for j in range(2):
            load_vec(g, scT[:, :, j:j + 1], g.inp["cs"][b, j], 8, w=["ada_sc"])
        S.dma("sp", brow[:], g.inp["b_ada"][l].partition_broadcast(2), w=["ada_brow"])
        load_vec(g, nw[:, :, 0:1], g.inp["norm_mix_w"][l], 8, w=["ada_nw"])
        load_vec(g, nw[:, :, 1:2], g.inp["norm_ffn_w"][l], 8, w=["ada_nw"])
        S.op("act", "activation", out=scT[:], in_=scT[:], func=AF.Silu, r=["ada_sc"], w=["ada_sc"])
        wsrc = g.inp["w_ada"][l].rearrange("(c p) f -> p c f", p=128)
        for pc in range(12):
            s = pc % 2
            bk = 1 + pc % 4
            S.dma("sp", wa[s][:], wsrc[:, :, pc * 512:(pc + 1) * 512], w=[("ada_w", s)])
            for kc in range(8):
                S.op("pe", "matmul", out=g.ps[bk][0:2, :], lhsT=scT[:, kc, :], rhs=wa[s][:, kc, :], start=(kc == 0), stop=(kc == 7),
                     r=[("ada_w", s), "ada_sc"], w=[("ps", bk)])
            S.op("dve", "tensor_tensor", out=mrow[:, pc * 512:(pc + 1) * 512], in0=g.ps[bk][0:2, :], in1=brow[:, pc * 512:(pc + 1) * 512], op=ALU.add,
                 r=[("ps", bk), "ada_brow"], w=[("ada_mrow", pc)])
        pm = g.ps[0]
        for fc in range(48):
            S.op("pe", "transpose", out=pm[:, fc * 2:fc * 2 + 2], in_=mrow[0:2, fc * 128:(fc + 1) * 128], identity=g.c["ident_f"][0:2, 0:2],
                 r=[("ada_mrow", fc // 4), ("c", "ident_f")], w=[("ps", 0)])
        S.op("dve", "tensor_copy", out=g.modT[:], in_=pm[:, 0:96].rearrange("p (f j) -> p f j", j=2), r=[("ps", 0)], w=["modT"])
        for which, dst, k in ((1, g.a1, 0), (4, g.a2, 1)):
            S.op("dve", "tensor_scalar", out=tmp[:], in0=g.modT[:, which * 8:(which + 1) * 8, :], scalar1=1.0, scalar2=None,
                 op0=ALU.add, r=["modT"], w=["ada_tmp"])
            S.op("dve", "tensor_tensor", out=dst[:], in0=tmp[:], in1=nw[:, :, k:k + 1].to_broadcast([128, 8, 2]), op=ALU.mult,
                 r=["ada_tmp", "ada_nw"], w=[("a", k)])


def phase_proj(g, l, b, need_ctx):
    S = g.S
    with ExitStack() as sc:
        win = g.win
        xg = [g.sb(f"pj_xg{i}", [128, 8, 512], F32, sc) for i in range(2)]
        sq = g.sb("pj_sq", [128, 8, 512], BF16, sc)
        rstd = g.sb("pj_rstd", [128, 512], F32, sc)
        tmp = [g.sb(f"pj_tmp{i}", [128, 512], F32, sc) for i in range(2)]
        xm = [g.sb(f"pj_xm{i}", [128, 8, 512], BF16, sc) for i in range(2)]
        ptok = [g.sb(f"pj_ptok{i}", [128, TOKC], F32, sc) for i in range(2)]
        pcT = g.sb("pj_pcT", [128, 4, 512], F32, sc)
        winr = [("pj_win", i) for i in range(4)]
        xsrc = g.xT_d[b].rearrange("(c p) t -> p c t", p=128)
        pcdst = g.pcT_d[b].rearrange("(c p) t -> p c t", p=128)
        colg = [(0, 512), (512, 512), (1024, 512), (1536, 512), (2048, 512), (2560, 16)]
        st = dict(nbank=0, tcount=0)

        def prologue(gi):
            t0, W, j = GROUPS[gi]
            s = gi % 2
            S.dma("sp", xg[s][:, :, 0:W], xsrc[:, :, t0:t0 + W], r=[("d", "xT", b)], w=[("pj_xg", s)])
            S.op("act", "activation", out=sq[:, :, 0:W], in_=xg[s][:, :, 0:W], func=AF.Square, r=[("pj_xg", s)], w=["pj_sq"])
            for kc in range(8):
                S.op("pe", "matmul", out=g.ps[0][:, 0:W], lhsT=g.c["ones_b"][:], rhs=sq[:, kc, 0:W], start=(kc == 0), stop=(kc == 7),
                     r=["pj_sq", ("c", "ones_b")], w=[("ps", 0)])
            S.op("dve", "tensor_scalar", out=rstd[:, 0:W], in0=g.ps[0][:, 0:W], scalar1=1.0 / D, scalar2=EPS, op0=ALU.mult, op1=ALU.add,
                 r=[("ps", 0)], w=["pj_rstd"])
            rsqrt_inplace(g, rstd[:, 0:W], ["pj_rstd"])
            for kc in range(8):
                ts = kc % 2
                S.op("dve", "tensor_tensor", out=tmp[ts][:, 0:W], in0=xg[s][:, kc, 0:W], in1=rstd[:, 0:W], op=ALU.mult,
                     r=[("pj_xg", s), "pj_rstd"], w=[("pj_tmp", ts)])
                S.op("act", "activation", out=xm[s][:, kc, 0:W], in_=tmp[ts][:, 0:W], func=AF.Identity, scale=g.a1[:, kc, j:j + 1],
                     bias=g.modT[:, 0 * 8 + kc, j:j + 1], r=[("pj_tmp", ts), ("a", 0), "modT"], w=[("pj_xm", s, kc)])

        def project(gi):
            t0, W, j = GROUPS[gi]
            s = gi % 2
            xmr = [("pj_xm", s, kc) for kc in range(8)]
            for tt in range(W // 128):
                ps_ = st["tcount"] % 2
                st["tcount"] += 1
                for ci, (c0, cw) in enumerate(colg):
                    bk = 1 + st["nbank"] % 7
                    st["nbank"] += 1
                    for kc in range(8):
                        S.op("pe", "matmul", out=g.ps[bk][:, 0:cw], lhsT=xm[s][:, kc, tt * 128:(tt + 1) * 128], rhs=win[:, kc, c0:c0 + cw],
                             start=(kc == 0), stop=(kc == 7), r=xmr + winr, w=[("ps", bk)])
                    evac(g, ci, ptok[ps_][:, c0:c0 + cw], g.ps[bk][:, 0:cw], r=[("ps", bk)], w=[("pj_ptok", ps_, ci)])
                S.dma("sp", g.ptok_d[b][t0 + tt * 128:t0 + (tt + 1) * 128, :], ptok[ps_][:],
                      r=[("pj_ptok", ps_, ci) for ci in range(6)], w=[("d", "ptok", b)])
            for fc in range(4):
                bk = 1 + st["nbank"] % 7
                st["nbank"] += 1
                for kc in range(8):
                    S.op("pe", "matmul", out=g.ps[bk][:, 0:W], lhsT=win[:, kc, TOKC + fc * 128:TOKC + (fc + 1) * 128], rhs=xm[s][:, kc, 0:W],
                         start=(kc == 0), stop=(kc == 7), r=xmr + winr, w=[("ps", bk)])
                evac(g, fc, pcT[:, fc, 0:W], g.ps[bk][:, 0:W], r=[("ps", bk)], w=[("pj_pcT", fc)])
            S.dma("sp", pcdst[:, :, t0:t0 + W], pcT[:, :, 0:W], r=[("pj_pcT", fc) for fc in range(4)], w=[("d", "pcT", b)])

        prologue(0)
        for gi in range(len(GROUPS)):
            if gi + 1 < len(GROUPS):
                prologue(gi + 1)
            project(gi)


def phase_conv(g, l, b, need_ctx):
    S = g.S
    with ExitStack() as sc:
        cw = g.sb("cv_w", [128, 2, 31], F32, sc)
        cb = g.sb("cv_b", [128, 2, 1], F32, sc)
        lnw = g.sb("cv_lnw", [128, 256], F32, sc)
        lnb = g.sb("cv_lnb", [128, 256], F32, sc)
        for c in range(2):
            S.dma("sp", cw[:, c, :], g.inp["conv_w"][l, :, c * 128:(c + 1) * 128].rearrange("j p -> p j"), w=["cv_w"])
        load_vec(g, cb[:], g.inp["conv_b"][l], 2, w=["cv_b"])
        S.dma("sp", lnw[:], g.inp["conv_ln_w"][l].partition_broadcast(128), w=["cv_lnw"])
        S.dma("sp", lnb[:], g.inp["conv_ln_b"][l].partition_broadcast(128), w=["cv_lnb"])
        at = [g.sb(f"cv_a{c}", [128, SEQ], F32, sc) for c in range(2)]
        gt = [g.sb(f"cv_g{c}", [128, SEQ], F32, sc) for c in range(2)]
        up = [g.sb(f"cv_u{c}", [128, SEQ + 30], F32, sc) for c in range(2)]
        yy = [g.sb(f"cv_y{c}", [128, SEQ], F32, sc) for c in range(2)]
        yt = [g.sb(f"cv_yt{i}", [128, 256], F32, sc) for i in range(2)]
        xc = [g.sb(f"cv_xc{i}", [128, 256], F32, sc) for i in range(2)]
        st = [g.sb(f"cv_st{i}", [128, 4], F32, sc) for i in range(2)]
        segs = ([(0, CTX)] if need_ctx else []) + [(CTX, SEQ)]
        nt = 0
        for (t0, n) in segs:
            for c in range(2):
                ve = "dve"
                S.dma("sp", at[c][:, 0:n], g.pcT_d[b][c * 128:(c + 1) * 128, t0:t0 + n], r=[("d", "pcT", b)], w=[("cv_a", c)])
                S.dma("sp", gt[c][:, 0:n], g.pcT_d[b][256 + c * 128:256 + (c + 1) * 128, t0:t0 + n], r=[("d", "pcT", b)], w=[("cv_g", c)])
                S.op("act", "activation", out=gt[c][:, 0:n], in_=gt[c][:, 0:n], func=AF.Sigmoid, r=[("cv_g", c)], w=[("cv_g", c)])
                S.op(ve, "memset", ap=up[c][:, 0:15], constant=0.0, w=[("cv_u", c)])
                S.op(ve, "memset", ap=up[c][:, 15 + n:30 + n], constant=0.0, r=[("cv_u", c)], w=[("cv_u", c)])
                S.op(ve, "tensor_tensor", out=up[c][:, 15:15 + n], in0=at[c][:, 0:n], in1=gt[c][:, 0:n], op=ALU.mult,
                     r=[("cv_a", c), ("cv_g", c), ("cv_u", c)], w=[("cv_u", c)])
                S.op(ve, "tensor_scalar", out=yy[c][:, 0:n], in0=up[c][:, 0:n], scalar1=cw[:, c, 0:1], scalar2=cb[:, c, :], op0=ALU.mult,
                     op1=ALU.add, r=[("cv_u", c), "cv_w", "cv_b"], w=[("cv_y", c)])
                for j in range(1, 31):
                    if ve == "dve":
                        S.op(ve, "scalar_tensor_tensor", out=yy[c][:, 0:n], in0=up[c][:, j:j + n], scalar=cw[:, c, j:j + 1], in1=yy[c][:, 0:n],
                             op0=ALU.mult, op1=ALU.add, r=[("cv_u", c), "cv_w", ("cv_y", c)], w=[("cv_y", c)])
                    else:
                        S.op(ve, "tensor_scalar", out=at[c][:, 0:n], in0=up[c][:, j:j + n], scalar1=cw[:, c, j:j + 1], scalar2=None, op0=ALU.mult,
                             r=[("cv_u", c), "cv_w", ("cv_a", c)], w=[("cv_a", c)])
                        S.op(ve, "tensor_tensor", out=yy[c][:, 0:n], in0=yy[c][:, 0:n], in1=at[c][:, 0:n], op=ALU.add,
                             r=[("cv_a", c), ("cv_y", c)], w=[("cv_y", c)])
            for tt in range(n // 128):
                i = nt % 2
                bk = nt % 8
                nt += 1
                for c in range(2):
                    S.op("pe", "transpose", out=g.ps[bk][:, c * 128:(c + 1) * 128], in_=yy[c][:, tt * 128:(tt + 1) * 128], identity=g.c["ident_f"][:],
                         r=[("cv_y", c), ("c", "ident_f")], w=[("ps", bk)])
                S.op("act", "activation", out=yt[i][:], in_=g.ps[bk][:, 0:256], func=AF.Copy, r=[("ps", bk)], w=[("cv_yt", i)])
                layer_norm_swish(g, yt[i], xc[i], st[i], lnw, lnb, ("cv_yt", i), ("cv_xc", i), ("cv_st", i))
                S.dma("sp", g.mix_d[b][t0 + tt * 128:t0 + (tt + 1) * 128, 768:1024], yt[i][:], r=[("cv_yt", i)], w=[("d", "mix", b)])


def layer_norm_swish(g, yt, xc, st, lnw, lnb, ky, kx, ks):
    S = g.S
    S.op("dve", "tensor_reduce", out=st[:, 0:1], in_=yt[:], axis=AX.X, op=ALU.add, r=[ky], w=[ks])
    S.op("dve", "tensor_scalar", out=st[:, 0:1], in0=st[:, 0:1], scalar1=-1.0 / 256, scalar2=None, op0=ALU.mult, r=[ks], w=[ks])
    S.op("dve", "tensor_scalar", out=xc[:], in0=yt[:], scalar1=st[:, 0:1], scalar2=None, op0=ALU.add, r=[ky, ks], w=[kx])
    S.op("dve", "tensor_tensor", out=yt[:], in0=xc[:], in1=xc[:], op=ALU.mult, r=[kx], w=[ky])
    S.op("dve", "tensor_reduce", out=st[:, 1:2], in_=yt[:], axis=AX.X, op=ALU.add, r=[ky], w=[ks])
    S.op("dve", "tensor_scalar", out=st[:, 1:2], in0=st[:, 1:2], scalar1=1.0 / 256, scalar2=EPS, op0=ALU.mult, op1=ALU.add, r=[ks], w=[ks])
    rsqrt_inplace(g, st[:, 1:2], [ks])
    S.op("dve", "tensor_scalar", out=xc[:], in0=xc[:], scalar1=st[:, 1:2], scalar2=None, op0=ALU.mult, r=[kx, ks], w=[kx])
    S.op("dve", "tensor_tensor", out=xc[:], in0=xc[:], in1=lnw[:], op=ALU.mult, r=[kx, "cv_lnw"], w=[kx])
    S.op("dve", "tensor_tensor", out=xc[:], in0=xc[:], in1=lnb[:], op=ALU.add, r=[kx, "cv_lnb"], w=[kx])
    S.op("act", "activation", out=yt[:], in_=xc[:], func=AF.Sigmoid, r=[kx], w=[ky])
    S.op("dve", "tensor_tensor", out=yt[:], in0=yt[:], in1=xc[:], op=ALU.mult, r=[kx, ky], w=[ky])


def phase_na(g, l, b, need_ctx):
    S, nc = g.S, g.nc
    with ExitStack() as sc:
        qw = g.sb("na_qw", [128, 64], F32, sc)
        kw_ = g.sb("na_kw", [128, 64], F32, sc)
        wbc = g.sb("na_wbc", [128, 16, 64], F32, sc)
        cm = g.sb("na_cmask", [128, 64], F32, sc)
        S.dma("sp", cm[:], g.cin["cmask"][:], w=[("c", "cmask")])
        etmp = g.sb("na_etmp", [128, 14, 64], F32, sc)
        etab = g.sb("na_etab", [128, 8, 14, 64], BF16, sc)
        qT = g.sb("na_qT", [128, 8, T], BF16, sc)
        kT = g.sb("na_kT", [128, 4, T], BF16, sc)
        vE = g.sb("na_vE", [128, NT, 8, 65], BF16, sc)
        vO = g.sb("na_vO", [128, 15, 8, 65], BF16, sc)
        pt = [g.sb(f"na_pt{i}", [128, 1536], F32, sc) for i in range(2)]
        sq = g.sb("na_sq", [128, 1024], F32, sc)
        ms = g.sb("na_ms", [128, 16], F32, sc)
        qkn = g.sb("na_qkn", [128, 1024], BF16, sc)
        pv = [g.sb(f"na_pv{i}", [128, 512], F32, sc) for i in range(2)]
        ex = [g.sb(f"na_ex{i}", [128, 384], BF16, sc) for i in range(3)]
        pm = [g.sb(f"na_pm{i}", [128, 256], BF16, sc) for i in range(3)]
        rd = [g.sb(f"na_rd{i}", [64, 8], F32, sc) for i in range(2)]
        ona = [g.sb(f"na_o{i}", [64, 8, 64], F32, sc) for i in range(2)]
        S.dma("sp", qw[:], g.inp["na_qnorm_w"][l].partition_broadcast(128), w=["na_qw"])
        S.dma("sp", kw_[:], g.inp["na_knorm_w"][l].partition_broadcast(128), w=["na_kw"])
        S.op("dve", "tensor_scalar", out=wbc[:, 0:8, :], in0=qw[:].unsqueeze(1).to_broadcast([128, 8, 64]), scalar1=0.125, scalar2=None,
             op0=ALU.mult, r=["na_qw"], w=["na_wbc"])
        S.op("dve", "tensor_copy", out=wbc[:, 8:16, :], in_=kw_[:].unsqueeze(1).to_broadcast([128, 8, 64]), r=["na_kw", "na_wbc"], w=["na_wbc"])
        S.op("pool", "memset", ap=vE[:, :, :, 64:65], constant=1.0, w=["na_vE1"])
        S.op("pool", "memset", ap=qT[:], constant=0.0, w=["na_qT"])
        S.op("pool", "memset", ap=vO[:, :, :, 64:65], constant=1.0, w=["na_vO1"])
        rp = g.inp["rpbT"]
        for h in range(8):
            src = bass.AP(rp, (l * 8 + h) * 960 * 64, [[64, 128], [64 * 64, 14], [1, 64]])
            S.dma("sp", etmp[:], src, w=["na_etmp"])
            S.op("act", "activation", out=etmp[:], in_=etmp[:], func=AF.Exp, r=["na_etmp"], w=["na_etmp"])
            S.op("dve", "tensor_tensor", out=etab[:, h, :, :], in0=etmp[:], in1=cm[:].unsqueeze(1).to_broadcast([128, 14, 64]), op=ALU.mult,
                 r=["na_etmp", ("c", "cmask")], w=[("na_etab", h)])
        for ti in range(NT):
            s = ti % 2
            S.dma("sp", pt[s][:], g.ptok_d[b][ti * 128:(ti + 1) * 128, B_Q:TOKC], r=[("d", "ptok", b)], w=[("na_pt", s)])
            S.op("act", "activation", out=sq[:], in_=pt[s][:, 0:1024], func=AF.Square, r=[("na_pt", s)], w=["na_sq"])
            S.op("dve", "tensor_reduce", out=ms[:], in_=sq[:].rearrange("p (h d) -> p h d", d=64), axis=AX.X, op=ALU.add, r=["na_sq"], w=["na_ms"])
            S.op("dve", "tensor_scalar", out=ms[:], in0=ms[:], scalar1=1.0 / 64, scalar2=EPS, op0=ALU.mult, op1=ALU.add, r=["na_ms"], w=["na_ms"])
            rsqrt_inplace(g, ms[:], ["na_ms"])
            S.op("dve", "tensor_tensor", out=sq[:].rearrange("p (h d) -> p h d", d=64), in0=pt[s][:, 0:1024].rearrange("p (h d) -> p h d", d=64),
                 in1=ms[:].unsqueeze(2).to_broadcast([128, 16, 64]), op=ALU.mult, r=[("na_pt", s), "na_ms", "na_sq"], w=["na_sq"])
            S.op("dve", "tensor_tensor", out=qkn[:].rearrange("p (h d) -> p h d", d=64), in0=sq[:].rearrange("p (h d) -> p h d", d=64),
                 in1=wbc[:], op=ALU.mult, r=["na_sq", "na_wbc"], w=["na_qkn"])
            bk = ti % 2
            pbf = g.ps[bk].bitcast(BF16)
            for j in range(8):
                S.op("pe", "transpose", out=pbf[:, j * 128:(j + 1) * 128], in_=qkn[:, j * 128:(j + 1) * 128], identity=g.c["ident_b"][:],
                     r=["na_qkn", ("c", "ident_b")], w=[("ps", bk)])
            S.op("act", "activation", out=qT[0:64, 0:8:2, ti * 128:(ti + 1) * 128], in_=pbf[0:64, 0:512].rearrange("p (j t) -> p j t", j=4), func=AF.Copy,
                 r=[("ps", bk), "na_qT"], w=["na_qT"])
            S.op("dve", "tensor_copy", out=qT[64:128, 1:8:2, ti * 128:(ti + 1) * 128], in_=pbf[64:128, 0:512].rearrange("p (j t) -> p j t", j=4),
                 r=[("ps", bk), "na_qT"], w=["na_qT"])
            S.op("dve", "tensor_copy", out=kT[:, :, ti * 128:(ti + 1) * 128], in_=pbf[:, 512:1024].rearrange("p (j t) -> p j t", j=4),
                 r=[("ps", bk)], w=["na_kT"])
            S.op("pool", "tensor_copy", out=vE[:, ti, :, 0:64], in_=pt[s][:, 1024:1536].rearrange("p (h d) -> p h d", d=64),
                 r=[("na_pt", s), "na_vE1"], w=["na_vE"])
        for j in range(15):
            s = j % 2
            r0 = CTX + 64 + j * 128
            S.dma("sp", pv[s][:], g.ptok_d[b][r0:r0 + 128, B_V:TOKC], r=[("d", "ptok", b)], w=[("na_pv", s)])
            S.op("pool", "tensor_copy", out=vO[:, j, :, 0:64], in_=pv[s][:].rearrange("p (h d) -> p h d", d=64), r=[("na_pv", s), "na_vO1"], w=["na_vO"])
        units = []
        if need_ctx:
            for qb in range(4):
                units.append((qb * 64, qb * 64, []))
        for r in range(32):
            r0 = min(max(r - 4, 0), 24)
            loc = []
            for j in range(4):
                gr = r0 + 2 * j
                vt = vE[:, 2 + gr // 2] if gr % 2 == 0 else vO[:, (gr - 1) // 2]
                loc.append((CTX + gr * 64, vt))
            units.append((CTX + r * 64, CTX + r * 64, loc, r0 - r + 7))
        nu = 0
        na_r = ["na_qT", "na_kT", "na_vE", "na_vO", "na_vE1", "na_vO1"]
        items = [(ui, h) for ui in range(len(units)) for h in range(8)]

        def tiles_of(ui):
            loc = units[ui][2]
            return [(k0, vt) for (k0, vt) in loc] + [(0, vE[:, 0]), (128, vE[:, 1])], len(loc)

        def scores(i):
            ui, h = items[i]
            tq = units[ui][0]
            pr = h // 2
            sb_, xi = i % 4, i % 3
            tiles, nl = tiles_of(ui)
            nk = len(tiles)
            for jj, (k0, vt) in enumerate(tiles):
                S.op("pe", "matmul", out=g.ps[sb_][:, jj * 64:(jj + 1) * 64], lhsT=kT[:, pr, k0:k0 + 128], rhs=qT[:, h, tq:tq + 64],
                     start=True, stop=True, r=na_r, w=[("ps", sb_)])
            S.op("act", "activation", out=ex[xi][:, 0:nk * 64], in_=g.ps[sb_][:, 0:nk * 64], func=AF.Exp, r=[("ps", sb_)], w=[("na_ex", xi)])
            if nl:
                d0 = units[ui][3]
                S.op("dve", "tensor_tensor", out=pm[xi][:].rearrange("p (j q) -> p j q", j=4), in0=ex[xi][:, 0:256].rearrange("p (j q) -> p j q", j=4),
                     in1=etab[:, h, d0:d0 + 7:2, :], op=ALU.mult, r=[("na_ex", xi), ("na_etab", h)], w=[("na_pm", xi)])

        def pv(i):
            ui, h = items[i]
            xi = i % 3
            ob = (4 + 2 * (ui % 2), 5 + 2 * (ui % 2))
            tiles, nl = tiles_of(ui)
            nk = len(tiles)
            pso = g.ps[ob[h // 4]]
            oc = (h % 4) * 65
            for jj, (k0, vt) in enumerate(tiles):
                lhs = pm[xi][:, jj * 64:(jj + 1) * 64] if jj < nl else ex[xi][:, jj * 64:(jj + 1) * 64]
                S.op("pe", "matmul", out=pso[0:64, oc:oc + 65], lhsT=lhs, rhs=vt[:, h, :], start=(jj == 0), stop=(jj == nk - 1),
                     r=[("na_ex", xi), ("na_pm", xi)] + na_r, w=[("ps", ob[h // 4])])
            if h == 7:
                trow = units[ui][1]
                oi = ui % 2
                for hh in range(2):
                    psq = g.ps[ob[hh]]
                    v3 = psq[0:64, 0:260].rearrange("p (h d) -> p h d", d=65)
                    S.op("dve", "reciprocal", out=rd[oi][:, hh * 4:(hh + 1) * 4], in_=v3[:, :, 64], r=[("ps", ob[hh])], w=[("na_rd", oi, hh)])
                    S.op("dve", "tensor_tensor", out=ona[oi][:, hh * 4:(hh + 1) * 4, :], in0=v3[:, :, 0:64],
                         in1=rd[oi][:, hh * 4:(hh + 1) * 4].unsqueeze(2).to_broadcast([64, 4, 64]), op=ALU.mult,
                         r=[("ps", ob[hh]), ("na_rd", oi, hh)], w=[("na_o", oi, hh)])
                S.dma("sp", g.mix_d[b][trow:trow + 64, 256:768], ona[oi][:].rearrange("p h d -> p (h d)"), r=[("na_o", oi, 0), ("na_o", oi, 1)],
                      w=[("d", "mix", b)])

        scores(0)
        for i in range(len(items)):
            if i + 1 < len(items):
                scores(i + 1)
            pv(i)


def phase_mlstm(g, l, b, need_ctx):
    S = g.S
    with ExitStack() as sc:
        qk = g.sb("ml_qk", [128, NT, 512], BF16, sc)
        g.c["tri"] = g.sb("ml_tri", [128, 8, 128], F32, sc)
        g.c["negmask"] = g.sb("ml_negmask", [128, 2, 512], F32, sc)
        S.dma("sp", g.c["tri"][:], g.cin["tri"][:], w=[("c", "tri")])
        S.dma("sp", g.c["negmask"][:], g.cin["negmask"][:], w=[("c", "negmask")])
        vx = g.sb("ml_vx", [128, NT, 4, 65], BF16, sc)
        osig = g.sb("ml_osig", [128, NT, 256], F32, sc)
        gl = g.sb("ml_gl", [128, NT, 16], F32, sc)
        cum = g.sb("ml_cum", [128, NT, 16], F32, sc)
        hh = [g.sb(f"ml_h{d}", [128, NT, 256], F32, sc) for d in range(2)]
        qkT = g.sb("ml_qkT", [128, NT, 4, 128], BF16, sc)
        qm = g.sb("ml_qm", [128, NT, 4, 128], BF16, sc)
        igb = g.sb("ml_igb", [128, 8], F32, sc)
        fgb = g.sb("ml_fgb", [128, 8], F32, sc)
        nwb = g.sb("ml_nwb", [128, 256], F32, sc)
        pt = [g.sb(f"ml_pt{i}", [128, 1040], F32, sc) for i in range(2)]
        rc = [g.sb(f"ml_rc{i}", [128, 256], F32, sc) for i in range(2)]
        rs = [g.sb(f"ml_rs{i}", [128, 256], F32, sc) for i in range(2)]
        t0_ = g.sb("ml_t0", [128, 16, 16], F32, sc)
        t1_ = g.sb("ml_t1", [128, 16, 16], F32, sc)
        t2_ = g.sb("ml_t2", [128, 16, 16], F32, sc)
        t3_ = g.sb("ml_t3", [128, 16, 16], F32, sc)
        zz = g.sb("ml_zz", [128, 8], F32, sc)
        S.dma("sp", igb[:], g.inp["mlstm_ig_b"][l].partition_broadcast(128), w=["ml_igb"])
        S.dma("sp", fgb[:], g.inp["mlstm_fg_b"][l].partition_broadcast(128), w=["ml_fgb"])
        S.dma("sp", nwb[:], g.inp["mlstm_norm_w"][l].partition_broadcast(128), w=["ml_nwb"])
        S.op("pool", "memset", ap=vx[:, :, :, 64:65], constant=1.0, w=["ml_vx1"])
        S.op("pool", "memset", ap=qm[:], constant=0.0, w=["ml_qm0"])
        for ti in range(NT):
            s = ti % 2
            S.dma("sp", pt[s][:], g.ptok_d[b][ti * 128:(ti + 1) * 128, 0:1040], r=[("d", "ptok", b)], w=[("ml_pt", s)])
            S.dma("sp", rc[s][:], g.cin["ropeC"][ti * 128:(ti + 1) * 128, :], w=[("ml_rc", s)])
            S.dma("sp", rs[s][:], g.cin["ropeS"][ti * 128:(ti + 1) * 128, :], w=[("ml_rs", s)])
            x4 = pt[s][:, 0:512].rearrange("p (g x i) -> p g x i", x=2, i=16)
            x0, x1 = x4[:, :, 0, :], x4[:, :, 1, :]
            C = rc[s][:].rearrange("p (g i) -> p g i", i=16)
            Sn = rs[s][:].rearrange("p (g i) -> p g i", i=16)
            o4 = qk[:, ti, :].rearrange("p (g x i) -> p g x i", x=2, i=16)
            kr = [("ml_pt", s), ("ml_rc", s), ("ml_rs", s)]
            S.op("dve", "tensor_tensor", out=t0_[:], in0=x0, in1=C, op=ALU.mult, r=kr, w=["ml_t0"])
            S.op("pool", "tensor_tensor", out=t1_[:], in0=x1, in1=Sn, op=ALU.mult, r=kr, w=["ml_t1"])
            S.op("dve", "tensor_tensor", out=o4[:, :, 0, :], in0=t0_[:], in1=t1_[:], op=ALU.subtract, r=["ml_t0", "ml_t1"], w=[("ml_qk", ti, 0)])
            S.op("pool", "tensor_tensor", out=t2_[:], in0=x1, in1=C, op=ALU.mult, r=kr, w=["ml_t2"])
            S.op("dve", "tensor_tensor", out=t3_[:], in0=x0, in1=Sn, op=ALU.mult, r=kr, w=["ml_t3"])
            S.op("pool", "tensor_tensor", out=o4[:, :, 1, :], in0=t2_[:], in1=t3_[:], op=ALU.add, r=["ml_t2", "ml_t3"], w=[("ml_qk", ti, 1)])
            S.op("pool", "tensor_copy", out=vx[:, ti, :, 0:64], in_=pt[s][:, 512:768].rearrange("p (h d) -> p h d", d=64),
                 r=[("ml_pt", s), "ml_vx1"], w=[("ml_vx", ti)])
            S.op("act", "activation", out=osig[:, ti, :], in_=pt[s][:, 768:1024], func=AF.Sigmoid, r=[("ml_pt", s)], w=[("ml_osig", ti)])
            S.op("dve", "tensor_tensor", out=gl[:, ti, 0:8], in0=pt[s][:, 1024:1032], in1=igb[:], op=ALU.add, r=[("ml_pt", s), "ml_igb"], w=[("ml_gl", ti)])
            S.op("dve", "tensor_tensor", out=zz[:], in0=pt[s][:, 1032:1040], in1=fgb[:], op=ALU.add, r=[("ml_pt", s), "ml_fgb"], w=["ml_zz"])
            S.op("act", "activation", out=zz[:], in_=zz[:], func=AF.Exp, scale=-1.0, r=["ml_zz"], w=["ml_zz"])
            S.op("act", "activation", out=zz[:], in_=zz[:], func=AF.Ln, bias=1.0, r=["ml_zz"], w=["ml_zz"])
            S.op("dve", "tensor_scalar", out=gl[:, ti, 8:16], in0=zz[:], scalar1=-1.0, scalar2=None, op0=ALU.mult, r=["ml_zz", ("ml_gl", ti)], w=[("ml_gl", ti)])
            bk = ti % 2
            pb = g.ps[bk]
            S.op("pe", "matmul", out=pb[:, 0:4], lhsT=g.c["tri"][:, 0, :], rhs=gl[:, ti, 8:12], start=True, stop=True, r=[("ml_gl", ti), ("c", "tri")], w=[("ps", bk)])
            S.op("pe", "matmul", out=pb[:, 4:8], lhsT=g.c["tri"][:, 4, :], rhs=gl[:, ti, 12:16], start=True, stop=True, r=[("ml_gl", ti), ("c", "tri")], w=[("ps", bk)])
            S.op("pe", "matmul", out=pb[:, 8:16], lhsT=g.c["ones_f"][:], rhs=gl[:, ti, 8:16], start=True, stop=True, r=[("ml_gl", ti), ("c", "ones_f")], w=[("ps", bk)])
            S.op("dve", "tensor_copy", out=cum[:, ti, :], in_=pb[:, 0:16], r=[("ps", bk)], w=[("ml_cum", ti)])
            bk2 = 2 + ti % 2
            pbf = g.ps[bk2].bitcast(BF16)
            for j in range(4):
                S.op("pe", "transpose", out=pbf[:, j * 128:(j + 1) * 128], in_=qk[:, ti, j * 128:(j + 1) * 128], identity=g.c["ident_b"][:],
                     r=[("ml_qk", ti, 0), ("ml_qk", ti, 1), ("c", "ident_b")], w=[("ps", bk2)])
            S.op("act", "activation", out=qkT[:, ti, :, :], in_=pbf[:, 0:512].rearrange("p (j t) -> p j t", j=4), func=AF.Copy, r=[("ps", bk2)], w=[("ml_qkT", ti)])
            S.op("act", "activation", out=qm[0:64, ti, 0:4:2, :], in_=pbf[0:64, 0:256].rearrange("p (j t) -> p j t", j=2), func=AF.Copy, r=[("ps", bk2), "ml_qm0"], w=[("ml_qm", ti, 0)])
            S.op("act", "activation", out=qm[64:128, ti, 1:4:2, :], in_=pbf[64:128, 0:256].rearrange("p (j t) -> p j t", j=2), func=AF.Copy, r=[("ps", bk2), "ml_qm0"], w=[("ml_qm", ti, 1)])
        sc4 = [[g.sb(f"ml_sc{d}{i}", [128, 16], F32, sc) for i in range(2)] for d in range(2)]
        rmat = [[g.sb(f"ml_rmat{d}{i}", [128, 4, 128], F32, sc) for i in range(2)] for d in range(2)]
        AT = [[g.sb(f"ml_AT{d}{i}", [128, 4, 128], F32, sc) for i in range(2)] for d in range(2)]
        Sm = [[g.sb(f"ml_Sm{d}{i}", [128, 4, 128], BF16, sc) for i in range(2)] for d in range(2)]
        qs = [[g.sb(f"ml_qs{d}{i}", [128, 256], BF16, sc) for i in range(2)] for d in range(2)]
        ks = [[g.sb(f"ml_ks{d}{i}", [128, 256], BF16, sc) for i in range(2)] for d in range(2)]
        qsT = [[g.sb(f"ml_qsT{d}{i}", [128, 2, 128], BF16, sc) for i in range(2)] for d in range(2)]
        Cf = [g.sb(f"ml_Cf{d}", [128, 2, 130], F32, sc) for d in range(2)]
        Cb = [[g.sb(f"ml_Cb{d}{i}", [128, 2, 130], BF16, sc) for i in range(2)] for d in range(2)]
        rd = [g.sb(f"ml_rd{d}", [128, 4], F32, sc) for d in range(2)]
        orders = [list(range(NT)), [1, 0] + list(range(NT - 1, 1, -1))]
        for d in range(2):
            S.op("pool", "memset", ap=Cf[d][:], constant=0.0, w=[("ml_Cf", d)])
            S.op("pool", "memset", ap=Cb[d][0][:], constant=0.0, w=[("ml_Cb", d, 0)])

        def pre(idx, d):
            ti = orders[d][idx]
            first = idx == 0
            i2 = idx % 2
            sc_ = sc4[d][i2]
            ksc = ("ml_sc", d, i2)
            bcol = cum[:, ti, d * 4:(d + 1) * 4]
            bL = cum[:, ti, 8 + d * 4:12 + d * 4]
            ig = gl[:, ti, d * 4:(d + 1) * 4]
            rr = [("ml_cum", ti), ("ml_gl", ti)]
            S.op("dve", "tensor_tensor", out=sc_[:, 4:8], in0=bL, in1=bcol, op=ALU.subtract, r=rr, w=[ksc])
            S.op("dve", "tensor_tensor", out=sc_[:, 4:8], in0=sc_[:, 4:8], in1=ig, op=ALU.add, r=rr + [ksc], w=[ksc])
            S.op("dve", "tensor_tensor", out=sc_[:, 8:12], in0=ig, in1=bcol, op=ALU.subtract, r=rr + [ksc], w=[ksc])
            S.op("dve", "tensor_copy", out=sc_[:, 0:4], in_=bcol, r=rr + [ksc], w=[ksc])
            S.op("dve", "tensor_copy", out=sc_[:, 12:16], in_=bL, r=rr + [ksc], w=[ksc])
            S.op("act", "activation", out=sc_[:, 0:8], in_=sc_[:, 0:8], func=AF.Exp, r=[ksc], w=[ksc])
            S.op("act", "activation", out=sc_[:, 12:16], in_=sc_[:, 12:16], func=AF.Exp, r=[ksc], w=[ksc])
            S.op("dve", "tensor_tensor", out=rmat[d][i2][:], in0=g.c["tri"][:, d * 4:(d + 1) * 4, :],
                 in1=gl[:, ti, 8 + d * 4:12 + d * 4].unsqueeze(2).to_broadcast([128, 4, 128]), op=ALU.mult, r=[("ml_gl", ti), ("c", "tri")], w=[("ml_rmat", d, i2)])
            ba = d
            S.op("pe", "matmul", out=g.ps[ba][:, :], lhsT=g.c["ones_f"][:], rhs=rmat[d][i2][:].rearrange("p h t -> p (h t)"), start=True, stop=False,
                 r=[("ml_rmat", d, i2), ("c", "ones_f")], w=[("ps", ba)])
            S.op("pe", "matmul", out=g.ps[ba][:, :], lhsT=g.c["ident_f"][:],
                 rhs=g.c["negmask"][:, d, :], start=False, stop=True, r=[("c", "negmask"), ("c", "ident_f")], w=[("ps", ba)])
            for h in range(4):
                S.op("act", "activation", out=AT[d][i2][:, h, :], in_=g.ps[ba][:, h * 128:(h + 1) * 128], func=AF.Exp, bias=sc_[:, 8 + h:9 + h],
                     r=[("ps", ba), ksc], w=[("ml_AT", d, i2)])
            bs = 2 + d
            for h in range(4):
                pr = h // 2
                S.op("pe", "matmul", out=g.ps[bs][:, h * 128:(h + 1) * 128], lhsT=qkT[:, ti, 2 + pr, :], rhs=qm[:, ti, h, :],
                     start=True, stop=True, r=[("ml_qkT", ti), ("ml_qm", ti, 0), ("ml_qm", ti, 1)], w=[("ps", bs)])
            S.op("dve", "tensor_tensor", out=Sm[d][i2][:], in0=g.ps[bs][:, :].rearrange("p (h t) -> p h t", h=4), in1=AT[d][i2][:], op=ALU.mult,
                 r=[("ps", bs), ("ml_AT", d, i2)], w=[("ml_Sm", d, i2)])
            qkr = [("ml_qk", ti, 0), ("ml_qk", ti, 1)]
            S.op("pool", "tensor_tensor", out=ks[d][i2][:].rearrange("p (h x) -> p h x", h=4), in0=qk[:, ti, 256:512].rearrange("p (h x) -> p h x", h=4),
                 in1=sc_[:, 4:8].unsqueeze(2).to_broadcast([128, 4, 64]), op=ALU.mult, r=qkr + [ksc], w=[("ml_ks", d, i2)])
            if not first:
                S.op("pool", "tensor_tensor", out=qs[d][i2][:].rearrange("p (h x) -> p h x", h=4), in0=qk[:, ti, 0:256].rearrange("p (h x) -> p h x", h=4),
                     in1=sc_[:, 0:4].unsqueeze(2).to_broadcast([128, 4, 64]), op=ALU.mult, r=qkr + [ksc], w=[("ml_qs", d, i2)])
                bt = 4
                pbf = g.ps[bt].bitcast(BF16)
                for j in range(2):
                    S.op("pe", "transpose", out=pbf[:, j * 128:(j + 1) * 128], in_=qs[d][i2][:, j * 128:(j + 1) * 128], identity=g.c["ident_b"][:],
                         r=[("ml_qs", d, i2), ("c", "ident_b")], w=[("ps", bt)])
                S.op("act", "activation", out=qsT[d][i2][:], in_=pbf[:, 0:256].rearrange("p (j t) -> p j t", j=2), func=AF.Copy, r=[("ps", bt)], w=[("ml_qsT", d, i2)])

        def post(idx, d):
            ti = orders[d][idx]
            first = idx == 0
            i2 = idx % 2
            sc_ = sc4[d][i2]
            ksc = ("ml_sc", d, i2)
            bc = 7
            for pr in range(2):
                S.op("pe", "matmul", out=g.ps[bc][:, pr * 130:(pr + 1) * 130], lhsT=ks[d][i2][:, pr * 128:(pr + 1) * 128],
                     rhs=vx[:, ti, 2 * pr:2 * pr + 2, :].rearrange("p h x -> p (h x)"), start=True, stop=True,
                     r=[("ml_ks", d, i2), ("ml_vx", ti), "ml_vx1"], w=[("ps", bc)])
            for h in range(4):
                pr, po = h // 2, (h % 2) * 64
                c0 = (h % 2) * 65
                S.op("dve", "scalar_tensor_tensor", out=Cf[d][po:po + 64, pr, c0:c0 + 65], in0=Cf[d][po:po + 64, pr, c0:c0 + 65],
                     scalar=sc_[po:po + 64, 12 + h:13 + h], in1=g.ps[bc][po:po + 64, pr * 130 + c0:pr * 130 + c0 + 65], op0=ALU.mult, op1=ALU.add,
                     r=[("ps", bc), ksc, ("ml_Cf", d)], w=[("ml_Cf", d)])
            S.op("act", "activation", out=Cb[d][(idx + 1) % 2][:], in_=Cf[d][:], func=AF.Copy, r=[("ml_Cf", d)], w=[("ml_Cb", d, (idx + 1) % 2)])
            bo = 5 + d
            for h in range(4):
                pr = h // 2
                S.op("pe", "matmul", out=g.ps[bo][:, h * 65:(h + 1) * 65], lhsT=Sm[d][i2][:, h, :], rhs=vx[:, ti, h, :], start=True, stop=first,
                     r=[("ml_Sm", d, i2), ("ml_vx", ti), "ml_vx1"], w=[("ps", bo)])
                if not first:
                    S.op("pe", "matmul", out=g.ps[bo][:, h * 65:(h + 1) * 65], lhsT=qsT[d][i2][:, pr, :],
                         rhs=Cb[d][i2][:, pr, (h % 2) * 65:(h % 2 + 1) * 65], start=False, stop=True, r=[("ml_qsT", d, i2), ("ml_Cb", d, i2)], w=[("ps", bo)])
            o3 = g.ps[bo][:, 0:260].rearrange("p (h x) -> p h x", x=65)
            S.op("act", "activation", out=rd[d][:], in_=o3[:, :, 64], func=AF.Abs, r=[("ps", bo)], w=[("ml_rd", d)])
            S.op("dve", "tensor_scalar", out=rd[d][:], in0=rd[d][:], scalar1=1.0, scalar2=None, op0=ALU.max, r=[("ml_rd", d)], w=[("ml_rd", d)])
            S.op("dve", "reciprocal", out=rd[d][:], in_=rd[d][:], r=[("ml_rd", d)], w=[("ml_rd", d)])
            S.op("dve", "tensor_tensor", out=hh[d][:, ti, :].rearrange("p (h x) -> p h x", h=4), in0=o3[:, :, 0:64],
                 in1=rd[d][:].unsqueeze(2).to_broadcast([128, 4, 64]), op=ALU.mult, r=[("ps", bo), ("ml_rd", d)], w=[("ml_h", d, ti)])

        for d in range(2):
            pre(0, d)
        for idx in range(NT):
            if idx + 1 < NT:
                for d in range(2):
                    pre(idx + 1, d)
            for d in range(2):
                post(idx, d)
        hs = [g.sb(f"ml_hs{i}", [128, 256], F32, sc) for i in range(2)]
        hq = [g.sb(f"ml_hq{i}", [128, 256], F32, sc) for i in range(2)]
        m4 = [g.sb(f"ml_m4{i}", [128, 4], F32, sc) for i in range(2)]
        for ti in range(0 if need_ctx else 2, NT):
            i = ti % 2
            S.op("dve", "tensor_tensor", out=hs[i][:], in0=hh[0][:, ti, :], in1=hh[1][:, ti, :], op=ALU.add, r=[("ml_h", 0, ti), ("ml_h", 1, ti)], w=[("ml_hs", i)])
            S.op("act", "activation", out=hq[i][:], in_=hs[i][:], func=AF.Square, r=[("ml_hs", i)], w=[("ml_hq", i)])
            S.op("dve", "tensor_reduce", out=m4[i][:], in_=hq[i][:].rearrange("p (h x) -> p h x", h=4), axis=AX.X, op=ALU.add, r=[("ml_hq", i)], w=[("ml_m4", i)])
            S.op("dve", "tensor_scalar", out=m4[i][:], in0=m4[i][:], scalar1=1.0 / 64, scalar2=EPS, op0=ALU.mult, op1=ALU.add, r=[("ml_m4", i)], w=[("ml_m4", i)])
            rsqrt_inplace(g, m4[i][:], [("ml_m4", i)])
            S.op("dve", "tensor_tensor", out=hs[i][:].rearrange("p (h x) -> p h x", h=4), in0=hs[i][:].rearrange("p (h x) -> p h x", h=4),
                 in1=m4[i][:].unsqueeze(2).to_broadcast([128, 4, 64]), op=ALU.mult, r=[("ml_hs", i), ("ml_m4", i)], w=[("ml_hs", i)])
            S.op("pool", "tensor_tensor", out=hs[i][:], in0=hs[i][:], in1=nwb[:], op=ALU.mult, r=[("ml_hs", i), "ml_nwb"], w=[("ml_hs", i)])
            S.op("pool", "tensor_tensor", out=hs[i][:], in0=hs[i][:], in1=osig[:, ti, :], op=ALU.mult, r=[("ml_hs", i), ("ml_osig", ti)], w=[("ml_hs", i)])
            S.dma("sp", g.mix_d[b][ti * 128:(ti + 1) * 128, 0:256], hs[i][:], r=[("ml_hs", i)], w=[("d", "mix", b)])


def phase_ffn(g, l, b, need_ctx):
    S = g.S
    ne = g.ne
    last = (l == DEPTH - 1)
    groups = GROUPS if need_ctx else GROUPS[1:]
    with ExitStack() as sc:
        xT = g.sb("ff_xT", [128, 8, T], F32, sc)
        hx = g.sb("ff_hx", [128, 8, T], BF16, sc)
        xsrc = g.xT_d[b].rearrange("(c p) t -> p c t", p=128)
        for gi, (t0, W, j) in enumerate(groups):
            S.dma("sp", xT[:, :, t0:t0 + W], xsrc[:, :, t0:t0 + W], r=[("d", "xT", b)], w=[("ff_xT", t0)])
        with ExitStack() as s2:
            wout = g.sb("ff_wout", [128, 8, D], BF16, s2)
            gateT = g.sb("ff_gateT", [32, T], F32, s2)
            b2s = g.sb("mo_b2", [32, D], F32, s2)
            rw = g.sb("ff_rw", [128, 8, NE], F32, s2)
            rb = g.sb("ff_rb", [128, NE], F32, s2)
            mt = [g.sb(f"ff_mt{i}", [128, D], F32, s2) for i in range(2)]
            mtb = [g.sb(f"ff_mtb{i}", [128, D], BF16, s2) for i in range(2)]
            mixT = [g.sb(f"ff_mixT{i}", [128, 8, 512], BF16, s2) for i in range(2)]
            sq = g.sb("ff_sq", [128, 8, 512], BF16, s2)
            rstd = g.sb("ff_rstd", [128, 512], F32, s2)
            tmp = [g.sb(f"ff_tmp{i}", [128, 512], F32, s2) for i in range(2)]
            hxf = g.sb("ff_hxf", [128, 8, 512], F32, s2)
            lg = [g.sb(f"ff_lg{i}", [128, NE], F32, s2) for i in range(2)]
            t8 = [g.sb(f"ff_t8{i}", [128, 8], F32, s2) for i in range(2)]
            ms = [g.sb(f"ff_ms{i}", [128, NE], F32, s2) for i in range(2)]
            s1 = [g.sb(f"ff_s1{i}", [128, 2], F32, s2) for i in range(2)]
            S.dma("pool", wout[:], g.inp["w_out"][l].rearrange("(c p) f -> p c f", p=128), w=["ff_wout"])
            S.dma("sp", rw[:], g.inp["router_w"][l].rearrange("(c p) f -> p c f", p=128), w=["ff_rw"])
            S.dma("sp", rb[:], g.inp["router_b"][l].partition_broadcast(128), w=["ff_rb"])
            stt = dict(nbk=0, ntile=0)

            def load_mix(gi):
                t0, W, j = groups[gi]
                ms_ = gi % 2
                for tt in range(W // 128):
                    i = stt["ntile"] % 2
                    stt["ntile"] += 1
                    S.dma("sp", mt[i][:], g.mix_d[b][t0 + tt * 128:t0 + (tt + 1) * 128, :], r=[("d", "mix", b)], w=[("ff_mt", i)])
                    S.op("act", "activation", out=mtb[i][:], in_=mt[i][:], func=AF.Copy, r=[("ff_mt", i)], w=[("ff_mtb", i)])
                    bk = stt["nbk"] % 8
                    stt["nbk"] += 1
                    pbf = g.ps[bk].bitcast(BF16)
                    for c in range(8):
                        S.op("pe", "transpose", out=pbf[:, c * 128:(c + 1) * 128], in_=mtb[i][:, c * 128:(c + 1) * 128], identity=g.c["ident_b"][:],
                             r=[("ff_mtb", i), ("c", "ident_b")], w=[("ps", bk)])
                    evac(g, tt, mixT[ms_][:, :, tt * 128:(tt + 1) * 128], pbf[:, :].rearrange("p (c t) -> p c t", c=8), r=[("ps", bk)], w=[("ff_mixT", ms_, tt)])

            def ffn_group(gi):
                t0, W, j = groups[gi]
                ms_ = gi % 2
                xk = ("ff_xT", t0)
                nbk = stt["nbk"]
                mr = [("ff_mixT", ms_, tt) for tt in range(W // 128)]
                for dc in range(8):
                    bk = nbk % 8
                    nbk += 1
                    for fc in range(8):
                        S.op("pe", "matmul", out=g.ps[bk][:, 0:W], lhsT=wout[:, fc, dc * 128:(dc + 1) * 128], rhs=mixT[ms_][:, fc, 0:W], start=(fc == 0), stop=(fc == 7),
                             r=mr + ["ff_wout"], w=[("ps", bk)])
                    S.op("dve", "scalar_tensor_tensor", out=xT[:, dc, t0:t0 + W], in0=g.ps[bk][:, 0:W], scalar=g.modT[:, 16 + dc, j:j + 1], in1=xT[:, dc, t0:t0 + W],
                         op0=ALU.mult, op1=ALU.add, r=[("ps", bk), "modT", xk], w=[xk])
                S.op("act", "activation", out=sq[:, :, 0:W], in_=xT[:, :, t0:t0 + W], func=AF.Square, r=[xk], w=["ff_sq"])
                bk = nbk % 8
                nbk += 1
                for kc in range(8):
                    S.op("pe", "matmul", out=g.ps[bk][:, 0:W], lhsT=g.c["ones_b"][:], rhs=sq[:, kc, 0:W], start=(kc == 0), stop=(kc == 7),
                         r=["ff_sq", ("c", "ones_b")], w=[("ps", bk)])
                S.op("dve", "tensor_scalar", out=rstd[:, 0:W], in0=g.ps[bk][:, 0:W], scalar1=1.0 / D, scalar2=EPS, op0=ALU.mult, op1=ALU.add,
                     r=[("ps", bk)], w=["ff_rstd"])
                rsqrt_inplace(g, rstd[:, 0:W], ["ff_rstd"])
                for kc in range(8):
                    ts = kc % 2
                    S.op("dve", "tensor_tensor", out=tmp[ts][:, 0:W], in0=xT[:, kc, t0:t0 + W], in1=rstd[:, 0:W], op=ALU.mult, r=[xk, "ff_rstd"], w=[("ff_tmp", ts)])
                    S.op("act", "activation", out=hxf[:, kc, 0:W], in_=tmp[ts][:, 0:W], func=AF.Identity, scale=g.a2[:, kc, j:j + 1], bias=g.modT[:, 24 + kc, j:j + 1],
                         r=[("ff_tmp", ts), ("a", 1), "modT"], w=[("ff_hxf", kc)])
                    S.op("pool", "tensor_copy", out=hx[:, kc, t0:t0 + W], in_=hxf[:, kc, 0:W], r=[("ff_hxf", kc)], w=[("ff_hx", t0)])
                hr = [("ff_hxf", kc) for kc in range(8)]
                for tt in range(W // 128):
                    i = tt % 2
                    bk = nbk % 8
                    nbk += 1
                    for kc in range(8):
                        S.op("pe", "matmul", out=g.ps[bk][:, 0:NE], lhsT=hxf[:, kc, tt * 128:(tt + 1) * 128], rhs=rw[:, kc, :], start=(kc == 0), stop=(kc == 7),
                             r=hr + ["ff_rw"], w=[("ps", bk)])
                    kl, k8, km, ks1 = ("ff_lg", i), ("ff_t8", i), ("ff_ms", i), ("ff_s1", i)
                    S.op("dve", "tensor_tensor", out=lg[i][:], in0=g.ps[bk][:, 0:NE], in1=rb[:], op=ALU.add, r=[("ps", bk), "ff_rb"], w=[kl])
                    S.op("dve", "max", out=t8[i][:], in_=lg[i][:], r=[kl], w=[k8])
                    S.op("dve", "tensor_scalar", out=ms[i][:], in0=lg[i][:], scalar1=t8[i][:, 3:4], scalar2=None, op0=ALU.is_ge, r=[kl, k8], w=[km])
                    S.op("dve", "tensor_scalar", out=s1[i][:, 0:1], in0=t8[i][:, 0:1], scalar1=-1.0, scalar2=None, op0=ALU.mult, r=[k8], w=[ks1])
                    S.op("act", "activation", out=lg[i][:], in_=lg[i][:], func=AF.Exp, bias=s1[i][:, 0:1], r=[kl, ks1], w=[kl])
                    S.op("dve", "tensor_tensor", out=lg[i][:], in0=lg[i][:], in1=ms[i][:], op=ALU.mult, r=[kl, km], w=[kl])
                    S.op("dve", "tensor_reduce", out=s1[i][:, 1:2], in_=lg[i][:], axis=AX.X, op=ALU.add, r=[kl, ks1], w=[ks1])
                    S.op("dve", "reciprocal", out=s1[i][:, 1:2], in_=s1[i][:, 1:2], r=[ks1], w=[ks1])
                    S.op("dve", "tensor_scalar", out=lg[i][:], in0=lg[i][:], scalar1=s1[i][:, 1:2], scalar2=None, op0=ALU.mult, r=[kl, ks1], w=[kl])
                    bk2 = nbk % 8
                    nbk += 1
                    S.op("pe", "transpose", out=g.ps[bk2][0:NE, 0:128], in_=lg[i][:], identity=g.c["ident_f"][:], r=[kl, ("c", "ident_f")], w=[("ps", bk2)])
                    S.op("act", "activation", out=gateT[:, t0 + tt * 128:t0 + (tt + 1) * 128], in_=g.ps[bk2][0:NE, 0:128], func=AF.Copy, r=[("ps", bk2)], w=[("ff_gateT", t0)])
                stt["nbk"] = nbk

            load_mix(0)
            for gi in range(len(groups)):
                if gi + 1 < len(groups):
                    load_mix(gi + 1)
                ffn_group(gi)
            nbk = stt["nbk"]
            S.dma("sp", b2s[:], g.inp["exp_b2"][l], w=["mo_b2"])
            for (t0, W, j) in groups:
                for dc in range(8):
                    bk = nbk % 8
                    nbk += 1
                    S.op("pe", "matmul", out=g.ps[bk][:, 0:W], lhsT=b2s[0:32, dc * 128:(dc + 1) * 128], rhs=gateT[0:32, t0:t0 + W], start=True, stop=True,
                         r=["mo_b2", ("ff_gateT", t0)], w=[("ps", bk)])
                    S.op("dve", "scalar_tensor_tensor", out=xT[:, dc, t0:t0 + W], in0=g.ps[bk][:, 0:W], scalar=g.modT[:, 40 + dc, j:j + 1], in1=xT[:, dc, t0:t0 + W],
                         op0=ALU.mult, op1=ALU.add, r=[("ps", bk), "modT", ("ff_xT", t0)], w=[("ff_xT", t0)])
            for (t0, W, j) in groups:
                S.dma("sp", g.gate_d[b][:, t0:t0 + W], gateT[:, t0:t0 + W], r=[("ff_gateT", t0)], w=[("d", "gate", b)])
        S.barrier()
        with ExitStack() as s3:
            w1 = [g.sb(f"mo_w1{i}", [128, 8, 2, 512], BF16, s3) for i in range(2)]
            w2 = [g.sb(f"mo_w2{i}", [128, 4, D], BF16, s3) for i in range(2)]
            b1T = g.sb("mo_b1T", [128, NE, 16], F32, s3)
            gbc = [g.sb(f"mo_gbc{i}", [128, T], F32, s3) for i in range(2)]
            gl_ = [g.sb(f"mo_gl{i}", [128, 512], F32, s3) for i in range(2)]
            sg_ = [g.sb(f"mo_sg{i}", [128, 512], F32, s3) for i in range(2)]
            ln_ = [g.sb(f"mo_ln{i}", [128, 512], F32, s3) for i in range(2)]
            act = [g.sb(f"mo_act{i}", [128, 4, 512], BF16, s3) for i in range(2)]
            for e0 in range(0, NE, 4):
                S.dma("sp", b1T[:, e0:e0 + 4, :], g.inp["exp_b1"][l, e0:e0 + 4, :].rearrange("e (c p) -> p e c", p=128), w=["mo_b1T"])
            S.op("dve", "tensor_scalar", out=b1T[:, :, 8:16], in0=b1T[:, :, 8:16], scalar1=1.0, scalar2=None, op0=ALU.add, r=["mo_b1T"], w=["mo_b1T"])
            nbk = 0
            steps = [(e, hf) for e in range(ne) for hf in range(2)]
            units = [(k, gi) for k in range(len(steps)) for gi in range(len(groups))]
            t00 = groups[0][0]

            def load_w(k):
                e, hf = steps[k]
                wi = k % 2
                if hf == 0:
                    S.dma("sp", gbc[e % 2][:, t00:T], g.gate_d[b][e, t00:T].partition_broadcast(128), r=[("d", "gate", b)], w=[("mo_gbc", e % 2)])
                w1src = g.inp["exp_w1"][l, e].rearrange("(c p) f -> p c f", p=128)
                for q2 in range(2):
                    S.dma("pool", w1[wi][:, :, q2, :], w1src[:, :, q2 * 1024 + hf * 512:q2 * 1024 + (hf + 1) * 512], w=[("mo_w1", wi, q2)])
                S.dma("pool", w2[wi][:], g.inp["exp_w2"][l, e, hf * 512:(hf + 1) * 512, :].rearrange("(c p) d -> p c d", p=128), w=[("mo_w2", wi)])

            def W1(u, jp):
                k, gi = units[u]
                e, hf = steps[k]
                wi, ei, ai = k % 2, e % 2, u % 2
                t0, W, j = groups[gi]
                pbanks = {}
                for jc in (2 * jp, 2 * jp + 1):
                    for q2 in range(2):
                        bq = (jc % 2) * 2 + q2
                        pbanks[(jc, q2)] = bq
                        for kc in range(8):
                            S.op("pe", "matmul", out=g.ps[bq][:, 0:W], lhsT=w1[wi][:, kc, q2, jc * 128:(jc + 1) * 128], rhs=hx[:, kc, t0:t0 + W],
                                 start=(kc == 0), stop=(kc == 7), r=[("mo_w1", wi, q2), ("ff_hx", t0)], w=[("ps", bq)])
                pr_ = [(jc, jc % 2, hf * 4 + jc) for jc in (2 * jp, 2 * jp + 1)]
                for jc, ii, fidx in pr_:
                    S.op("dve", "tensor_scalar", out=gl_[ii][:, 0:W], in0=g.ps[pbanks[(jc, 0)]][:, 0:W], scalar1=b1T[:, e, fidx:fidx + 1], scalar2=7.0,
                         op0=ALU.add, op1=ALU.min, r=[("ps", pbanks[(jc, 0)]), "mo_b1T"], w=[("mo_gl", ii)])
                for jc, ii, fidx in pr_:
                    S.op("act", "activation", out=sg_[ii][:, 0:W], in_=gl_[ii][:, 0:W], func=AF.Sigmoid, scale=1.702, r=[("mo_gl", ii)], w=[("mo_sg", ii)])
                for jc, ii, fidx in pr_:
                    S.op("dve", "tensor_scalar", out=ln_[ii][:, 0:W], in0=g.ps[pbanks[(jc, 1)]][:, 0:W], scalar1=b1T[:, e, 8 + fidx:9 + fidx], scalar2=8.0,
                         op0=ALU.add, op1=ALU.min, r=[("ps", pbanks[(jc, 1)]), "mo_b1T"], w=[("mo_ln", ii)])
                for jc, ii, fidx in pr_:
                    S.op("dve", "tensor_tensor", out=gl_[ii][:, 0:W], in0=gl_[ii][:, 0:W], in1=sg_[ii][:, 0:W], op=ALU.mult,
                         r=[("mo_gl", ii), ("mo_sg", ii)], w=[("mo_gl", ii)])
                for jc, ii, fidx in pr_:
                    S.op("dve", "scalar_tensor_tensor", out=gl_[ii][:, 0:W], in0=ln_[ii][:, 0:W], scalar=-6.0, in1=gl_[ii][:, 0:W], op0=ALU.max, op1=ALU.mult,
                         r=[("mo_gl", ii), ("mo_ln", ii)], w=[("mo_gl", ii)])
                for jc, ii, fidx in pr_:
                    S.op("pool", "tensor_tensor", out=act[ai][:, jc, 0:W], in0=gl_[ii][:, 0:W], in1=gbc[ei][:, t0:t0 + W], op=ALU.mult,
                         r=[("mo_gl", ii), ("mo_gbc", ei)], w=[("mo_act", ai, jc)])

            w2cnt = [0]

            def W2(u, dcs):
                k, gi = units[u]
                wi, ai = k % 2, u % 2
                t0, W, j = groups[gi]
                ar = [("mo_act", ai, jc) for jc in range(4)]
                for dc in dcs:
                    bk = 4 + w2cnt[0] % 4
                    w2cnt[0] += 1
                    for jc in range(4):
                        S.op("pe", "matmul", out=g.ps[bk][:, 0:W], lhsT=w2[wi][:, jc, dc * 128:(dc + 1) * 128], rhs=act[ai][:, jc, 0:W], start=(jc == 0), stop=(jc == 3),
                             r=ar + [("mo_w2", wi)], w=[("ps", bk)])
                    S.op("dve", "scalar_tensor_tensor", out=xT[:, dc, t0:t0 + W], in0=g.ps[bk][:, 0:W], scalar=g.modT[:, 40 + dc, j:j + 1], in1=xT[:, dc, t0:t0 + W],
                         op0=ALU.mult, op1=ALU.add, r=[("ps", bk), "modT", ("ff_xT", t0)], w=[("ff_xT", t0)])

            load_w(0)
            W1(0, 0)
            W1(0, 1)
            for u in range(len(units)):
                k, gi = units[u]
                if gi == 0 and k + 1 < len(steps):
                    load_w(k + 1)
                nxt = u + 1 < len(units)
                if nxt:
                    W1(u + 1, 0)
                W2(u, range(0, 4))
                if nxt:
                    W1(u + 1, 1)
                W2(u, range(4, 8))
        S.barrier()
        if not last:
            for (t0, W, j) in groups:
                S.dma("sp", xsrc[:, :, t0:t0 + W], xT[:, :, t0:t0 + W], r=[("ff_xT", t0)], w=[("d", "xT", b)])
        else:
            with ExitStack() as s4:
                ot = [g.sb(f"fo_t{i}", [128, D], F32, s4) for i in range(2)]
                nbk = 0
                for ti in range(2, NT):
                    i = ti % 2
                    t0g = 256 + ((ti * 128 - 256) // 512) * 512
                    for h2 in range(2):
                        bk = nbk % 8
                        nbk += 1
                        for c4 in range(4):
                            c = h2 * 4 + c4
                            S.op("pe", "transpose", out=g.ps[bk][:, c4 * 128:(c4 + 1) * 128], in_=xT[:, c, ti * 128:(ti + 1) * 128], identity=g.c["ident_f"][:],
                                 r=[("ff_xT", t0g), ("c", "ident_f")], w=[("ps", bk)])
                        evac(g, h2, ot[i][:, h2 * 512:(h2 + 1) * 512], g.ps[bk][:, :], r=[("ps", bk)], w=[("fo_t", i, h2)])
                    S.dma("sp", g.y[b, (ti - 2) * 128:(ti - 1) * 128, :], ot[i][:], r=[("fo_t", i, 0), ("fo_t", i, 1)], w=[("d", "y")])


_CONSTS = None


def make_in_map(inp, core):
    global _CONSTS
    if _CONSTS is None:
        _CONSTS = _consts()
    b0 = 2 * core
    m = dict(_CONSTS)
    f = lambda a: np.ascontiguousarray(np.asarray(a, dtype=np.float32))
    m["x"] = f(inp["x"][b0:b0 + 2])
    m["ctx"] = f(inp["ctx"][b0:b0 + 2])
    cc = np.asarray(inp["c_ctx"], np.float32)
    m["cs"] = f(np.stack([np.stack([np.asarray(inp["c"][b0 + i], np.float32), cc]) for i in range(2)]))
    for k in ("norm_mix_w", "norm_ffn_w", "w_ada", "b_ada", "w_in", "mlstm_norm_w", "na_qnorm_w", "na_knorm_w", "conv_w", "conv_b",
              "conv_ln_w", "conv_ln_b", "w_out", "router_w", "router_b", "exp_w1", "exp_b1", "exp_w2", "exp_b2"):
        m[k] = f(inp[k])
    m["mlstm_ig_b"] = f(np.asarray(inp["mlstm_ig_b"]).reshape(DEPTH, 8))
    m["mlstm_fg_b"] = f(np.asarray(inp["mlstm_fg_b"]).reshape(DEPTH, 8))
    col = np.arange(64)
    dc = np.clip(col[:, None] - col[None, :] + 15, 0, 30)
    rp = np.asarray(inp["na_rpb"], np.float32)[:, :, :, dc]
    m["rpbT"] = f(rp.reshape(DEPTH, 8, 15 * 64, 64))
    return m


_NC_CACHE = {}


def kernel(**inputs):
    if "nc" not in _NC_CACHE:
        _NC_CACHE["nc"] = build_program()
    nc = _NC_CACHE["nc"]
    in_maps = [make_in_map(inputs, c) for c in range(8)]
    res = run_bass_kernel_spmd(nc, in_maps, core_ids=list(range(8)))
    out = np.concatenate([np.asarray(r["y"], dtype=np.float32) for r in res.results], axis=0)
    return out
```
